# Optimizing a Trainium2 kernel written in Bass

```python
import math
import jax, jax.numpy as jnp
from jax import lax
import numpy as np

D_MODEL = 1024
BATCH = 8
SEQ = 8192
DEPTH = 2

MIX_WIDTH = D_MODEL
CONV_CH = 512
CONV_WIDTH = 31
RET_HEADS = 4
RET_HEAD_DIM = 128
RET_WIDTH = RET_HEADS * RET_HEAD_DIM
RET_CHUNK = 128
ROPE_BASE = 10000.0
D_FF = 4 * D_MODEL
PLE_DIM = 256
IN_COLS = 2 * CONV_CH + 4 * RET_WIDTH
RMS_EPS = 1e-6
LN_EPS = 1e-5

kernel_name = "hybrid_conformer_conv_retention_block"


def rms_norm(x, g):
    xf = x.astype(jnp.float32)
    y = xf * lax.rsqrt(jnp.mean(xf * xf, axis=-1, keepdims=True) + RMS_EPS)
    return (y * g.astype(jnp.float32)).astype(x.dtype)


def layer_norm_f32(x, g, b):
    xf = x.astype(jnp.float32)
    mu = jnp.mean(xf, axis=-1, keepdims=True)
    var = jnp.mean(jnp.square(xf - mu), axis=-1, keepdims=True)
    return (xf - mu) * lax.rsqrt(var + LN_EPS) * g.astype(jnp.float32) + b.astype(jnp.float32)


def rotary(t, positions):
    half = t.shape[-1] // 2
    inv_freq = 1.0 / (ROPE_BASE ** (jnp.arange(0, half, dtype=jnp.float32) * (2.0 / t.shape[-1])))
    ang = positions.astype(jnp.float32)[..., None] * inv_freq
    cos = jnp.cos(ang)[:, :, None, :]
    sin = jnp.sin(ang)[:, :, None, :]
    t1, t2 = t[..., :half], t[..., half:]
    return jnp.concatenate([t1 * cos - t2 * sin, t2 * cos + t1 * sin], axis=-1)


def conv_module(u, dw_w, dw_b, ln_g, ln_b):
    a, b = jnp.split(u, 2, axis=-1)
    h = a * jax.nn.sigmoid(b)
    h = lax.conv_general_dilated(
        h, dw_w[:, None, :].astype(h.dtype), window_strides=(1,),
        padding=[(CONV_WIDTH - 1, 0)],
        dimension_numbers=('NWC', 'WIO', 'NWC'),
        feature_group_count=CONV_CH) + dw_b
    h = layer_norm_f32(h, ln_g, ln_b)
    return jax.nn.silu(h).astype(u.dtype)


def chunkwise_retention(q, k, v, positions):
    B, S = q.shape[0], q.shape[1]
    n_chunks = S // RET_CHUNK
    shp = (B, S, RET_HEADS, RET_HEAD_DIM)
    qf = rotary(q.astype(jnp.float32).reshape(shp), positions)
    kf = rotary(k.astype(jnp.float32).reshape(shp), positions) * (RET_HEAD_DIM ** -0.5)
    vf = v.astype(jnp.float32).reshape(shp)

    def to_chunks(t):
        return t.reshape(B, n_chunks, RET_CHUNK, RET_HEADS, RET_HEAD_DIM).transpose(1, 0, 3, 2, 4)

    qc, kc, vc = to_chunks(qf), to_chunks(kf), to_chunks(vf)

    log_g = jnp.log(1.0 - jnp.exp2(-5.0 - jnp.arange(RET_HEADS, dtype=jnp.float32)))
    idx = jnp.arange(RET_CHUNK, dtype=jnp.float32)
    diff = idx[:, None] - idx[None, :]
    decay_mask = jnp.where(diff[None] >= 0,
                           jnp.exp(jnp.maximum(diff, 0.0)[None] * log_g[:, None, None]), 0.0)
    zeta = jnp.exp((RET_CHUNK - 1 - idx)[None, :] * log_g[:, None])
    xi = jnp.exp((idx + 1.0)[None, :] * log_g[:, None])
    chunk_decay = jnp.exp(RET_CHUNK * log_g)

    def step(state, inp):
        qi, ki, vi = inp
        scores = jnp.einsum('bhqd,bhkd->bhqk', qi, ki) * decay_mask
        inner = jnp.einsum('bhqk,bhkd->bhqd', scores, vi)
        cross = jnp.einsum('bhqd,bhde->bhqe', qi, state) * xi[None, :, :, None]
        new_state = state * chunk_decay[None, :, None, None] + jnp.einsum(
            'bhkd,bhke->bhde', ki * zeta[None, :, :, None], vi)
        return new_state, inner + cross

    state0 = jnp.zeros((B, RET_HEADS, RET_HEAD_DIM, RET_HEAD_DIM), jnp.float32)
    _, out = lax.scan(step, state0, (qc, kc, vc))
    return out.transpose(1, 0, 3, 2, 4).reshape(shp)


def setup_inputs(seed: int = 0) -> dict:
    key = jax.random.key(seed)
    ks = jax.random.split(key, 20)
    f32 = jnp.float32

    def nrm(k, shape, scale):
        return jax.random.normal(k, shape, f32) * scale

    def gain(k, shape):
        return 1.0 + 0.02 * jax.random.normal(k, shape, f32)

    x = jax.random.normal(ks[0], (BATCH, SEQ, D_MODEL), f32)
    p = jax.random.normal(ks[1], (DEPTH, BATCH, SEQ, PLE_DIM), f32)
    positions = jnp.broadcast_to(jnp.arange(SEQ, dtype=jnp.int32)[None, :], (BATCH, SEQ))
    return {
        "x": x,
        "p": p,
        "positions": positions,
        "norm_mix_g": gain(ks[2], (DEPTH, D_MODEL)),
        "w_in": nrm(ks[3], (DEPTH, D_MODEL, IN_COLS), D_MODEL ** -0.5),
        "conv_dw_w": nrm(ks[4], (DEPTH, CONV_WIDTH, CONV_CH), CONV_WIDTH ** -0.5),
        "conv_dw_b": nrm(ks[5], (DEPTH, CONV_CH), 0.02),
        "conv_ln_g": gain(ks[6], (DEPTH, CONV_CH)),
        "conv_ln_b": nrm(ks[7], (DEPTH, CONV_CH), 0.02),
        "ret_gn_g": gain(ks[8], (DEPTH, RET_WIDTH)),
        "ret_gn_b": nrm(ks[9], (DEPTH, RET_WIDTH), 0.02),
        "w_out": nrm(ks[10], (DEPTH, MIX_WIDTH, D_MODEL), MIX_WIDTH ** -0.5),
        "norm_ffn_g": gain(ks[11], (DEPTH, D_MODEL)),
        "w_ff1": nrm(ks[12], (DEPTH, D_MODEL, D_FF), D_MODEL ** -0.5),
        "w_ff2": nrm(ks[13], (DEPTH, D_FF, D_MODEL), D_FF ** -0.5),
        "norm_ple_g": gain(ks[14], (DEPTH, D_MODEL)),
        "w_ple_gate": nrm(ks[15], (DEPTH, D_MODEL, D_MODEL), D_MODEL ** -0.5),
        "w_ple_proj": nrm(ks[16], (DEPTH, PLE_DIM, D_MODEL), PLE_DIM ** -0.5),
        "final_norm_g": gain(ks[17], (D_MODEL,)),
    }


def reference(x, p, positions, norm_mix_g, w_in, conv_dw_w, conv_dw_b, conv_ln_g, conv_ln_b,
              ret_gn_g, ret_gn_b, w_out, norm_ffn_g, w_ff1, w_ff2, norm_ple_g,
              w_ple_gate, w_ple_proj, final_norm_g):
    B, S = x.shape[0], x.shape[1]
    splits = [2 * CONV_CH, 2 * CONV_CH + RET_WIDTH, 2 * CONV_CH + 2 * RET_WIDTH,
              2 * CONV_CH + 3 * RET_WIDTH]
    for i in range(DEPTH):
        h = rms_norm(x, norm_mix_g[i])
        z = h @ w_in[i]
        u_conv, q, k, v, g = jnp.split(z, splits, axis=-1)
        conv_out = conv_module(u_conv, conv_dw_w[i], conv_dw_b[i], conv_ln_g[i], conv_ln_b[i])
        ret = chunkwise_retention(q, k, v, positions)
        mu = jnp.mean(ret, axis=-1, keepdims=True)
        var = jnp.mean(jnp.square(ret - mu), axis=-1, keepdims=True)
        ret = ((ret - mu) * lax.rsqrt(var + LN_EPS)).reshape(B, S, RET_WIDTH)
        ret = ret * ret_gn_g[i].astype(jnp.float32) + ret_gn_b[i].astype(jnp.float32)
        ret_out = (jax.nn.silu(g.astype(jnp.float32)) * ret).astype(x.dtype)
        mix = jnp.concatenate([conv_out, ret_out], axis=-1) @ w_out[i]
        x = x + mix
        h = rms_norm(x, norm_ffn_g[i])
        x = x + jnp.square(jax.nn.relu(h @ w_ff1[i])) @ w_ff2[i]
        h = rms_norm(x, norm_ple_g[i])
        x = x + jax.nn.sigmoid(h @ w_ple_gate[i]) * (p[i] @ w_ple_proj[i])
    return rms_norm(x, final_norm_g)
```

```python
import math
from contextlib import ExitStack

import numpy as np
import concourse.bass as bass
import concourse.mybir as mybir
from concourse.bass_utils import run_bass_kernel_spmd

F32 = mybir.dt.float32
BF16 = mybir.dt.bfloat16
I32 = mybir.dt.int32
AF = mybir.ActivationFunctionType
ALU = mybir.AluOpType

D = 1024
KC = 8
T = 512
NCH = T // 128
H = 4
CW = 31
HALO = CW - 1
PLE = 256
DFF = 4096
NPIECE = 29
SEQ = 8192
BATCH = 8
DEPTH = 2
RMS_EPS = 1e-6
LN_EPS = 1e-5
PL = 168
NPAR = 2 * PL + 8
C_INVF, C_SIGN, C_XI, C_DMASK, C_ZTAB, C_IDENT = 0, 1, 2, 6, 6 + 512, 6 + 1024
C_PSWAP = 6 + 1024 + 128
NCONST = 6 + 1024 + 256
PI = math.pi
TWO_PI = 2.0 * math.pi


class Tracker:
    ENG = ("pe", "act", "dve", "pool", "sp")

    def __init__(self, nc, es):
        self.nc = nc
        self.es = es
        self.streams = {e: [] for e in self.ENG}
        self.sem = {}
        self.cnt = {}
        self.waited = {e: {} for e in self.ENG}
        self.state = {}
        self.engsem = {}
        for e in ("pe", "act", "dve", "pool"):
            self.engsem[e] = self.new_sem("e_" + e)
        self.engsem["sp"] = None
        self.nwait = 0
        self.nops = 0
        self.label = ''
        self.pe_labels = []

    def new_sem(self, name):
        self.sem[name] = self.es.enter_context(self.nc.semaphore(name))
        self.cnt[name] = 0
        return name

    def _waits(self, eng, r, w):
        raw, oth = {}, {}

        def add(d, sv):
            s, v = sv
            if d.get(s, 0) < v:
                d[s] = v

        for k in r:
            st = self.state.get(k)
            if st and st[0]:
                add(raw, st[0])
            if st and isinstance(k, tuple) and k[0] == "ps":
                for s, v in st[1].items():
                    add(oth, (s, v))
        for k in w:
            st = self.state.get(k)
            if st:
                if st[0]:
                    add(oth, st[0])
                for s, v in st[1].items():
                    add(oth, (s, v))
        own = self.engsem[eng]
        need = dict(raw)
        for s, v in oth.items():
            if s == own:
                continue
            if need.get(s, 0) < v:
                need[s] = v
        wd = self.waited[eng]
        for s, v in need.items():
            if wd.get(s, 0) >= v:
                continue
            wd[s] = v
            self.streams[eng].append(("w", s, v))
            self.nwait += 1

    def _commit(self, r, w, sem, val):
        for k in w:
            self.state[k] = [(sem, val), {}]
        for k in r:
            st = self.state.get(k)
            if st is None:
                st = self.state[k] = [None, {}]
            st[1][sem] = val

    def op(self, eng, fn, r=(), w=()):
        self._waits(eng, r, w)
        sem = self.engsem[eng]
        self.cnt[sem] += 1
        self.streams[eng].append(("o", fn, sem, 1))
        self._commit(r, w, sem, self.cnt[sem])
        self.nops += 1

    def group(self, fns, r=(), w=()):
        self._waits("pe", r, w)
        sem = self.engsem["pe"]
        self.cnt[sem] += 1
        self.pe_labels.extend([self.label] * len(fns))
        allr = list(r)
        n = len(fns)
        for i, f in enumerate(fns):
            if isinstance(f, tuple):
                f, ri = f
                self._waits("pe", ri, ())
                allr.extend(ri)
            if i < n - 1:
                self.streams["pe"].append(("o", f, None, 0))
            else:
                self.streams["pe"].append(("o", f, sem, 1))
        self._commit(allr, w, sem, self.cnt[sem])
        self.nops += n

    def dma(self, eng, fn, sem, r=(), w=()):
        self._waits(eng, r, w)
        self.cnt[sem] += 16
        self.streams[eng].append(("o", fn, sem, 16))
        self._commit(r, w, sem, self.cnt[sem])

    def final_wait(self, eng, sem):
        self.streams[eng].append(("w", sem, self.cnt[sem]))

    def replay(self, block):
        def run(name):
            def f(e):
                for it in self.streams[name]:
                    if it[0] == "w":
                        e.wait_ge(self.sem[it[1]], it[2])
                    else:
                        ins = it[1](e)
                        if it[2] is not None:
                            ins.then_inc(self.sem[it[2]], it[3])
            return f

        block.tensor(run("pe"))
        block.scalar(run("act"))
        block.vector(run("dve"))
        block.gpsimd(run("pool"))
        block.sync(run("sp"))


def _pieces_src(W, l, pi):
    w_in, w_out, w_ff1, w_ff2, w_pg, w_pp = W

    def std(mat, r0, r1, c0):
        return mat[l, r0:r1, c0:c0 + 512].rearrange("(kc p) j -> p kc j", p=128)

    if pi < 8:
        cols = {0: 0, 1: 512, 2: 1024, 3: 1024, 4: 1536, 5: 1536, 6: 2048, 7: 2560}[pi]
        if pi in (3, 5):
            v = w_in[l, :, cols:cols + 512].rearrange("(kc p) (h two d) -> p kc h two d", p=128, two=2, d=64)
            return [("swap", (kc, 1 - tw), v[:, kc, :, tw, :]) for kc in range(8) for tw in range(2)]
        return [("std", None, std(w_in, 0, D, cols))]
    if pi < 10:
        return [("std", None, std(w_out, 0, D, (pi - 8) * 512))]
    if pi < 18:
        return [("std", None, std(w_ff1, 0, D, (pi - 10) * 512))]
    if pi < 26:
        half, jb = divmod(pi - 18, 4)
        return [("std", None, std(w_ff2, jb * 1024, (jb + 1) * 1024, half * 512))]
    if pi < 28:
        return [("std", None, std(w_pg, 0, D, (pi - 26) * 512))]
    return [("proj", None, w_pp[l].rearrange("(kc p) j -> p kc j", p=128))]


CAST_GROUPS = [[1, 0], [2], [4], [6, 7], [8, 9], [10, 11, 12, 13], [14, 15, 16, 17],
               [18, 19, 20, 21], [22, 23, 24, 25], [26, 27, 28]]


def I(method, *a, **kw):
    return lambda e: getattr(e, method)(*a, **kw)


STAGE = 99
DUMP_LABELS = None
FLAGS = set()


def build(S, depth=DEPTH):
    NT = S // T
    nc = bass.Bass("TRN2", target_bir_lowering=False)
    dt = nc.dram_tensor
    x_d = dt("x", [S, D], F32, kind="ExternalInput").ap()
    p_d = dt("p", [DEPTH, S, PLE], F32, kind="ExternalInput").ap()
    pos_d = dt("pos", [1, S], I32, kind="ExternalInput").ap()
    par_d = dt("params", [128, NPAR], F32, kind="ExternalInput").ap()
    con_d = dt("consts", [128, NCONST], F32, kind="ExternalInput").ap()
    W = (dt("w_in", [DEPTH, D, 3072], F32, kind="ExternalInput").ap(),
         dt("w_out", [DEPTH, D, D], F32, kind="ExternalInput").ap(),
         dt("w_ff1", [DEPTH, D, DFF], F32, kind="ExternalInput").ap(),
         dt("w_ff2", [DEPTH, DFF, D], F32, kind="ExternalInput").ap(),
         dt("w_pg", [DEPTH, D, D], F32, kind="ExternalInput").ap(),
         dt("w_pp", [DEPTH, PLE, D], F32, kind="ExternalInput").ap())
    out_d = dt("out", [S, D], F32, kind="ExternalOutput").ap()
    wscr = dt("wscr", [DEPTH, NPIECE, 128, 4096], BF16, kind="Internal").ap()

    es = ExitStack()
    with es:
        tk = Tracker(nc, es)
        sb = lambda name, shape, d: es.enter_context(nc.sbuf_tensor(name, shape, d))
        xT = sb("xT", [128, KC, T], F32)
        sq = sb("sq", [128, KC, T], BF16)
        hT = sb("hT", [128, KC, T], BF16)
        mixT = sb("mixT", [128, KC, T], BF16)
        Sst = sb("Sst", [128, DEPTH, 512], F32)
        Sbf = sb("Sbf", [128, DEPTH, 512], BF16)
        diag = sb("diag", [128, CW * 4, 128], BF16)
        cos2 = sb("cos2", [128, T], F32)
        sin2 = sb("sin2", [128, T], F32)
        NSLOT = 4
        ring = [sb(f"ring{i}", [128, 4096], BF16) for i in range(NSLOT)]
        par = sb("par", [128, NPAR], F32)
        con = sb("con", [128, NCONST], F32)
        whalf = sb("whalf", [128, DEPTH, CW * 4], F32)
        identb = sb("identb", [128, 128], BF16)
        ones1k = sb("ones1k", [128, 128], BF16)
        ones512 = sb("ones512", [128, 128], BF16)
        halo = sb("halo", [128, DEPTH, 4, 32], BF16)
        stats = sb("stats", [128, 4, 4, 6], F32)
        mv = sb("mv", [128, 4, 4, 2], F32)
        rstd4 = sb("rstd4", [128, 4, 4], F32)
        veps = sb("veps", [128, 4, 4], F32)
        nmr = sb("nmr", [128, 4, 4], F32)
        mhalf = sb("mhalf", [128, 4], F32)
        sdn = sb("sdn", [128, T], F32)
        tabw = sb("tabw", [128, 2], F32)
        pswap = sb("pswap", [128, 128], BF16)
        epsr = sb("epsr", [128, 2], F32)
        uoff = [0]
        U_BYTES = 81 * 1024
        U = sb("U", [128, U_BYTES // 4], F32)

        class UB:
            def __init__(s, nbytes, dtype, shape3=None, phase_off=None):
                if phase_off is not None:
                    uoff[0] = phase_off
                s.off = uoff[0]
                s.nbytes = nbytes
                uoff[0] += (nbytes + 1023) // 1024 * 1024
                assert uoff[0] <= U_BYTES, (uoff[0], U_BYTES)
                v = U[:, s.off // 4:(s.off + nbytes) // 4]
                if dtype != F32:
                    v = v.bitcast(dtype)
                if shape3 is not None:
                    v = v.rearrange("p (a b) -> p a b", a=shape3[0])
                    s.nch = shape3[0]
                else:
                    s.nch = 1
                s.ap = v

            def k(s, i=None, n=1):
                if i is None:
                    lo, hi = s.off, s.off + s.nbytes
                else:
                    cb = s.nbytes // s.nch
                    lo, hi = s.off + i * cb, s.off + (i + n) * cb
                return [("U", g) for g in range(lo // 1024, (hi + 1023) // 1024)]

        class PB:
            def __init__(s, name, shape, dtype):
                s.t = sb(name, shape, dtype)
                s.ap = s.t
                s.name = name

            def k(s, i=None, n=1):
                return [(s.name, i)] if i is not None else [(s.name, j) for j in range(s.t.shape[1])]
        qb = PB("qb", [128, 2, 512], BF16)
        hglu = UB(4 * 544 * 2, BF16, (4, 544), phase_off=0)
        th = UB(2 * 512 * 4, F32, (2, 512))
        qT = UB(4 * 512 * 2, BF16, (4, 512))
        kT = UB(4 * 512 * 2, BF16, (4, 512))
        t1 = UB(2 * 512 * 4, F32, (2, 512))
        t2 = UB(2 * 512 * 4, F32, (2, 512))
        vtok = UB(4 * 512 * 2, BF16, (4, 512))
        sgT = UB(4 * 512 * 4, F32, (4, 512))
        kz = UB(2 * 512 * 2, BF16, (2, 512))
        smk = UB(2 * 512 * 2, BF16, (2, 512))
        innsb = UB(2 * 512 * 4, F32, (2, 512))
        retsb = UB(2 * 512 * 4, F32, (2, 512))
        rn = UB(2 * 512 * 2, BF16, (2, 512))
        gtmp = UB(2 * 512 * 4, F32, (2, 512))
        ycv = UB(4 * 512 * 4, F32, (4, 512))
        ybf = UB(4 * 512 * 2, BF16, (4, 512))
        ysq = UB(4 * 512 * 2, BF16, (4, 512))
        ytmp = UB(2 * 512 * 4, F32, (2, 512))
        msb = UB(512 * 4, F32)
        varb = UB(512 * 4, F32)
        sdb = UB(512 * 4, F32)
        hid = UB(32 * 512 * 2, BF16, (32, 512), phase_off=0)
        sqf = UB(2 * 512 * 4, F32, (2, 512))
        thg = UB(2 * 512 * 4, F32, (2, 512))
        pltmp = UB(2 * 512 * 4, F32, (2, 512))
        p_in = UB(4 * 256 * 4, F32, (4, 256))
        pT = UB(2 * 512 * 2, BF16, (2, 512))
        xstage = UB(4 * 1024 * 4, F32, (4, 1024), phase_off=50 * 1024)
        posi = UB(512 * 4, I32)
        ang = UB(512 * 4, F32)
        kfb = UB(512 * 4, F32)
        kib = UB(512 * 4, I32)
        rr = UB(512 * 4, F32)
        wa = UB(512 * 4, F32)
        wb_ = UB(512 * 4, F32)
        ostage = UB(4 * 1024 * 4, F32, (4, 1024), phase_off=0)
        obuf = UB(2 * 512 * 4, F32, (2, 512))

        ps = [es.enter_context(nc.psum_tensor(f"ps{i}", [128, 512], F32)) for i in range(8)]
        psb = [p[:, :].bitcast(BF16) for p in ps]
        bank_ctr = [0]

        def nb():
            b = bank_ctr[0] % 8
            bank_ctr[0] += 1
            return b

        PK = lambda b: [("ps", b)]
        s_const = tk.new_sem("d_const")
        s_ring = [tk.new_sem(f"d_ring{i}") for i in range(NSLOT)]
        s_x = tk.new_sem("d_x")
        s_o = tk.new_sem("d_o")
        s_p = tk.new_sem("d_p")
        s_pos = tk.new_sem("d_pos")
        NG = len(CAST_GROUPS)
        NCS = 4
        s_cs = [tk.new_sem(f"d_cs{i}") for i in range(NCS)]
        piece_keys = {}

        def MM(out, lhsT, rhs, start, stop):
            return lambda e: e.matmul(out, lhsT=lhsT, rhs=rhs, start=start, stop=stop)

        def TR(out, in_, ident):
            return lambda e: e.transpose(out=out, in_=in_, identity=ident)

        tk.dma("sp", I("dma_start", out=par[:], in_=par_d), s_const, w=["par"])
        tk.dma("sp", I("dma_start", out=con[:], in_=con_d), s_const, w=["con"])
        fin = (s_const, tk.cnt[s_const])
        tk.state["par"] = [fin, {}]
        tk.state["con"] = [fin, {}]
        identf = con[:, C_IDENT:C_IDENT + 128]
        tk.op("dve", I("tensor_copy", out=identb[:], in_=identf), r=["con"], w=["identb"])
        tk.op("dve", I("tensor_copy", out=pswap[:], in_=con[:, C_PSWAP:C_PSWAP + 128]), r=["con"], w=["pswap"])
        tk.op("dve", I("memset", ones1k[:], 1.0 / 1024.0), w=["ones1k"])
        tk.op("dve", I("memset", ones512[:], 1.0 / 512.0), w=["ones512"])
        tk.op("dve", I("memset", Sst[:], 0.0), w=["S0", "S1"])
        tk.op("dve", I("memset", Sbf[:], 0.0), w=["Sbf0", "Sbf1"])
        tk.op("dve", I("memset", halo[:], 0.0), w=["halo0", "halo1"])
        tk.op("dve", I("memset", mhalf[:], -0.5), w=["mhalf"])
        tk.op("dve", I("memset", tabw[:], 1.0), w=["tabw"])
        tk.op("dve", I("memset", epsr[:, 0:1], RMS_EPS), w=["epsr"])
        tk.op("dve", I("memset", epsr[:, 1:2], LN_EPS), w=["epsr"])
        for l in range(depth):
            tk.op("dve", I("tensor_scalar", out=whalf[:, l, :], in0=par[:, l * PL + 44:l * PL + 168],
                           scalar1=0.5, scalar2=None, op0=ALU.mult), r=["par"], w=[("whalf", l)])

        cast_ptr = [0]
        cast_list = [(l, g) for l in range(depth) for g in range(NG)]

        cast_dma_ctr = [0]

        def emit_casts(upto):
            if 'nocast' in FLAGS:
                return
            while cast_ptr[0] < min(upto, len(cast_list)):
                l, g = cast_list[cast_ptr[0]]
                cast_ptr[0] += 1
                for pi in CAST_GROUPS[g]:
                    dst = wscr[l, pi]
                    deps = []
                    for kind, sw, src in _pieces_src(W, l, pi):
                        if kind == "std":
                            dv = dst.rearrange("p (kc j) -> p kc j", kc=8)
                        elif kind == "proj":
                            dv = dst[:, 0:2048].rearrange("p (kc j) -> p kc j", kc=2)
                        else:
                            dv = dst.rearrange("p (kc h two d) -> p kc h two d", kc=8, h=4, two=2)[:, sw[0], :, sw[1], :]
                        sl_ = cast_dma_ctr[0] % NCS
                        cast_dma_ctr[0] += 1
                        sem = s_cs[sl_]
                        key = ("wscr", l, pi, len(deps))
                        tk.dma("pool", I("dma_start", out=dv, in_=src), sem, r=[], w=[("castslot", sl_), key])
                        deps.append(key)
                    piece_keys[(l, pi)] = deps

        piece_group = {pi: g for g, lst in enumerate(CAST_GROUPS) for pi in lst}
        ring_ctr = [0]

        def ring_load(l, pi):
            emit_casts(l * NG + piece_group[pi] + 1)
            s = ring_ctr[0] % NSLOT
            ring_ctr[0] += 1
            n = 2048 if pi == 28 else 4096
            tk.dma("sp", I("dma_start", out=ring[s][:, 0:n], in_=wscr[l, pi][:, 0:n]), s_ring[s],
                   r=piece_keys[(l, pi)], w=[("ring", s)])
            return s

        def pc(l, name, n):
            base = {"gmix": 0, "gffn": 8, "gple": 16, "dwb": 24, "lng": 28, "lnb": 32, "gng": 36, "gnb": 40}[name]
            c = l * PL + base + n
            return par[:, c:c + 1]

        def emit_diag(l):
            if 'nodiag' in FLAGS:
                return
            for j in range(CW):
                for cc in range(4):
                    idx = j * 4 + cc
                    tk.op("pool", I("tensor_scalar", out=diag[:, idx, :], in0=identb[:], scalar1=whalf[:, l, idx:idx + 1],
                                    scalar2=0.0, op0=ALU.mult, op1=ALU.add),
                          r=["identb", ("whalf", l)], w=[("diag", idx)])

        def emit_rstd():
            b = nb()
            tk.group([(MM(ps[b][:, :], ones1k[:], sq[:, kc, :], kc == 0, kc == KC - 1), [("sq", kc)]) for kc in range(KC)],
                     r=["ones1k"], w=PK(b))
            tk.op("act", I("activation", out=sdn[:], in_=ps[b][:, :], func=AF.Ln, bias=epsr[:, 0:1], scale=1.0), r=PK(b) + ["epsr"], w=["sdn"])
            tk.op("act", I("activation", out=ps[b][:, :], in_=sdn[:], func=AF.Exp, scale=-0.5), r=["sdn"], w=PK(b))
            return b

        def emit_norm(l, gname):
            b = emit_rstd()
            for kc in range(KC):
                tk.op("dve", I("scalar_tensor_tensor", out=hT[:, kc, :], in0=xT[:, kc, :], scalar=pc(l, gname, kc),
                               in1=ps[b][:, :], op0=ALU.mult, op1=ALU.mult),
                      r=[("xT", kc), "par"] + PK(b), w=[("hT", kc)])

        def linear_fm(slot, rhs_fn, rhs_keys, nk, evac, mi_list=range(4)):
            for mi in mi_list:
                b = nb()
                tk.group([(MM(ps[b][:, :], ring[slot][:, kc * 512 + mi * 128: kc * 512 + mi * 128 + 128], rhs_fn(kc), kc == 0, kc == nk - 1), [rhs_keys[kc]])
                          for kc in range(nk)], r=[("ring", slot)], w=PK(b))
                evac(mi, b)

        def linear_first(pairs, rhs_fn, rhs_keys, nk, evacs):
            banks = [nb() for _ in pairs]
            fns = []
            for kc in range(nk):
                for (slot, mi), b in zip(pairs, banks):
                    fns.append((MM(ps[b][:, :], ring[slot][:, kc * 512 + mi * 128: kc * 512 + mi * 128 + 128], rhs_fn(kc), kc == 0, kc == nk - 1),
                                [rhs_keys[kc]]))
            tk.group(fns, r=[("ring", s) for s in sorted({p[0] for p in pairs})], w=[("ps", b) for b in banks])
            for ev, ((slot, mi), b) in zip(evacs, zip(pairs, banks)):
                ev(mi, b)

        def residual_evac(m, b):
            tk.op("dve", I("tensor_tensor", out=xT[:, m, :], in0=xT[:, m, :], in1=ps[b][:, :], op=ALU.add),
                  r=[("xT", m)] + PK(b), w=[("xT", m)])
            tk.op("act", I("activation", out=sq[:, m, :], in_=xT[:, m, :], func=AF.Square), r=[("xT", m)], w=[("sq", m)])

        hT_keys = [("hT", kc) for kc in range(KC)]
        rhs_h = lambda kc: hT[:, kc, :]
        CD = [float(np.exp(128.0 * np.log(1.0 - 2.0 ** (-5.0 - h)))) for h in range(H)]
        C1 = 6.28125
        C2 = TWO_PI - C1

        def emit_tile_prefetch(ti):
            t0 = ti * T
            tk.dma("sp", I("dma_start", out=posi.ap, in_=pos_d[0:1, t0:t0 + T].partition_broadcast(128)), s_pos, w=posi.k())
            tk.op("dve", I("tensor_copy", out=ang.ap, in_=posi.ap), r=posi.k(), w=ang.k())
            tk.op("dve", I("tensor_scalar", out=ang.ap, in0=ang.ap, scalar1=con[:, C_INVF:C_INVF + 1], scalar2=None, op0=ALU.mult),
                  r=ang.k() + ["con"], w=ang.k())
            tk.op("dve", I("tensor_scalar", out=kfb.ap, in0=ang.ap, scalar1=1.0 / TWO_PI, scalar2=None, op0=ALU.mult), r=ang.k(), w=kfb.k())
            tk.op("dve", I("tensor_copy", out=kib.ap, in_=kfb.ap), r=kfb.k(), w=kib.k())
            tk.op("dve", I("tensor_copy", out=kfb.ap, in_=kib.ap), r=kib.k(), w=kfb.k())
            tk.op("dve", I("scalar_tensor_tensor", out=rr.ap, in0=kfb.ap, scalar=-C1, in1=ang.ap, op0=ALU.mult, op1=ALU.add),
                  r=kfb.k() + ang.k(), w=rr.k())
            tk.op("dve", I("scalar_tensor_tensor", out=rr.ap, in0=kfb.ap, scalar=-C2, in1=rr.ap, op0=ALU.mult, op1=ALU.add),
                  r=kfb.k() + rr.k(), w=rr.k())
            for (shift, dst, key, scl) in ((PI / 2, cos2, "cos2", None), (0.0, sin2, "sin2", con[:, C_SIGN:C_SIGN + 1])):
                tk.op("dve", I("tensor_scalar", out=wa.ap, in0=rr.ap, scalar1=shift, scalar2=None, op0=ALU.add), r=rr.k(), w=wa.k())
                tk.op("dve", I("tensor_scalar", out=wb_.ap, in0=wa.ap, scalar1=PI, scalar2=TWO_PI, op0=ALU.is_gt, op1=ALU.mult), r=wa.k(), w=wb_.k())
                tk.op("dve", I("tensor_tensor", out=wa.ap, in0=wa.ap, in1=wb_.ap, op=ALU.subtract), r=wa.k() + wb_.k(), w=wa.k())
                tk.op("dve", I("tensor_scalar", out=wb_.ap, in0=wa.ap, scalar1=-PI, scalar2=TWO_PI, op0=ALU.is_lt, op1=ALU.mult), r=wa.k(), w=wb_.k())
                tk.op("dve", I("tensor_tensor", out=wa.ap, in0=wa.ap, in1=wb_.ap, op=ALU.add), r=wa.k() + wb_.k(), w=wa.k())
                tk.op("dve", I("tensor_scalar", out=wa.ap, in0=wa.ap, scalar1=3.1415925, scalar2=-3.1415925, op0=ALU.min, op1=ALU.max), r=wa.k(), w=wa.k())
                if scl is None:
                    tk.op("act", I("activation", out=dst[:], in_=wa.ap, func=AF.Sin), r=wa.k(), w=[key])
                else:
                    tk.op("act", I("activation", out=dst[:], in_=wa.ap, func=AF.Sin, scale=scl), r=wa.k() + ["con"], w=[key])
            tk.op("act", I("activation", out=tabw[:, 0:1], in_=tabw[:, 1:2], func=AF.Ln), r=[], w=["tabw"])
            tk.dma("sp", I("dma_start", out=xstage.ap, in_=x_d[t0:t0 + T, :].rearrange("(c p) f -> p c f", p=128)), s_x, w=xstage.k())

        emit_casts(4)
        emit_diag(0)
        emit_tile_prefetch(0)
        for ti in range(NT):
            t0 = ti * T
            for m in range(KC if 'min0' not in FLAGS else 0):
                b = nb()
                tk.group([TR(ps[b][:, c * 128:(c + 1) * 128], xstage.ap[:, c, m * 128:(m + 1) * 128], identf) for c in range(NCH)],
                         r=xstage.k() + ["con"], w=PK(b))
                tk.op("dve", I("tensor_copy", out=xT[:, m, :], in_=ps[b][:, :]), r=PK(b), w=[("xT", m)])
                tk.op("act", I("activation", out=sq[:, m, :], in_=ps[b][:, :], func=AF.Square), r=PK(b), w=[("sq", m)])

            for l in range(depth if STAGE >= 2 else 0):
                first = (ti == 0)
                gi = l * NG
                if first:
                    emit_casts(gi + 5)
                tk.label = 'norm1'
                emit_norm(l, "gmix")
                tk.op("pool", I("tensor_copy", out=hglu.ap[:, :, 0:HALO], in_=halo[:, l, :, 0:HALO]), r=[f"halo{l}"], w=hglu.k())
                tk.label = 'win_ab'
                sl_b = ring_load(l, 1)
                sl_a = ring_load(l, 0)
                for mi in range(4):
                    def ev_b(mi_, b):
                        tk.op("act", I("activation", out=th.ap[:, mi_ % 2, :], in_=ps[b][:, :], func=AF.Tanh, scale=0.5), r=PK(b), w=th.k(mi_ % 2))

                    def ev_a(mi_, b):
                        tk.op("dve", I("scalar_tensor_tensor", out=hglu.ap[:, mi_, HALO:HALO + T], in0=th.ap[:, mi_ % 2, :], scalar=1.0,
                                       in1=ps[b][:, :], op0=ALU.add, op1=ALU.mult), r=th.k(mi_ % 2) + PK(b), w=hglu.k(mi_))
                    if mi == 0:
                        linear_first([(sl_b, 0), (sl_a, 0), (sl_b, 1), (sl_a, 1)], rhs_h, hT_keys, KC, [ev_b, ev_a, ev_b, ev_a])
                    elif mi >= 2:
                        linear_fm(sl_b, rhs_h, hT_keys, KC, ev_b, mi_list=[mi])
                        linear_fm(sl_a, rhs_h, hT_keys, KC, ev_a, mi_list=[mi])
                tk.op("pool", I("tensor_copy", out=halo[:, l, :, 0:HALO], in_=hglu.ap[:, :, T:T + HALO]), r=hglu.k(), w=[f"halo{l}"])
                def conv_group(cc, l=l):
                    tk.label = 'conv'
                    b = nb()
                    tk.group([MM(ps[b][:, :], diag[:, j * 4 + cc, :], hglu.ap[:, cc, j:j + T], j == 0, j == CW - 1) for j in range(CW)],
                             r=[("diag", j * 4 + cc) for j in range(CW)] + hglu.k(cc), w=PK(b))
                    tk.op("act", I("activation", out=ycv.ap[:, cc, :], in_=ps[b][:, :], func=AF.Identity, bias=pc(l, "dwb", cc), scale=1.0),
                          r=PK(b) + ["par"], w=ycv.k(cc))
                    tk.op("act", I("activation", out=ybf.ap[:, cc, :], in_=ps[b][:, :], func=AF.Identity, bias=pc(l, "dwb", cc), scale=1.0),
                          r=PK(b) + ["par"], w=ybf.k(cc))
                    tk.op("act", I("activation", out=ysq.ap[:, cc, :], in_=ycv.ap[:, cc, :], func=AF.Square), r=ycv.k(cc), w=ysq.k(cc))

                def conv_tail_A():
                    tk.label = 'convstat'
                    bm = nb()
                    tk.group([(MM(ps[bm][:, :], ones512[:], ybf.ap[:, cc, :], cc == 0, cc == 3), ybf.k(cc)) for cc in range(4)], r=["ones512"], w=PK(bm))
                    be = nb()
                    tk.group([(MM(ps[be][:, :], ones512[:], ysq.ap[:, cc, :], cc == 0, cc == 3), ysq.k(cc)) for cc in range(4)], r=["ones512"], w=PK(be))
                    tk.op("act", I("activation", out=msb.ap, in_=ps[bm][:, :], func=AF.Copy), r=PK(bm), w=msb.k())
                    tk.op("act", I("activation", out=varb.ap, in_=ps[be][:, :], func=AF.Copy), r=PK(be), w=varb.k())
                    tk.op("pool", I("tensor_tensor", out=sdb.ap, in0=msb.ap, in1=msb.ap, op=ALU.mult), r=msb.k(), w=sdb.k())
                    tk.op("pool", I("tensor_tensor", out=varb.ap, in0=varb.ap, in1=sdb.ap, op=ALU.subtract), r=varb.k() + sdb.k(), w=varb.k())

                def conv_tail_B():
                    tk.op("act", I("activation", out=sdb.ap, in_=varb.ap, func=AF.Ln, bias=epsr[:, 1:2], scale=1.0), r=varb.k() + ["epsr"], w=sdb.k())
                    tk.op("act", I("activation", out=sdb.ap, in_=sdb.ap, func=AF.Exp, scale=-0.5), r=sdb.k(), w=sdb.k())

                def conv_tail_C():
                    for cc in range(4):
                        tk.op("pool", I("tensor_tensor", out=ycv.ap[:, cc, :], in0=ycv.ap[:, cc, :], in1=msb.ap, op=ALU.subtract),
                              r=ycv.k(cc) + msb.k(), w=ycv.k(cc))
                        tk.op("pool", I("tensor_tensor", out=ycv.ap[:, cc, :], in0=ycv.ap[:, cc, :], in1=sdb.ap, op=ALU.mult),
                              r=ycv.k(cc) + sdb.k(), w=ycv.k(cc))

                def conv_tail_D(l=l):
                    for cc in range(4):
                        tk.op("act", I("activation", out=mixT[:, cc, :], in_=ycv.ap[:, cc, :], func=AF.Silu, scale=pc(l, "lng", cc), bias=pc(l, "lnb", cc)),
                              r=ycv.k(cc) + ["par"], w=[("mixT", cc)])

                if first:
                    emit_casts(gi + 8)
                gcount = 0
                for (p_main, p_sw, dstT) in ((2, 3, qT), (4, 5, kT)):
                    sl_m = ring_load(l, p_main)
                    for h in range(H):
                        tk.label = 'win_qk'

                        def ev_m(mi_, b, dstT=dstT):
                            u = mi_ % 2
                            tk.op("act", I("activation", out=qb.ap[:, u, :], in_=ps[b][:, :], func=AF.Copy), r=PK(b), w=qb.k(u))
                            tk.op("dve", I("tensor_tensor", out=t1.ap[:, u, :], in0=ps[b][:, :], in1=cos2[:], op=ALU.mult),
                                  r=PK(b) + ["cos2"], w=t1.k(u))
                            b2 = nb()
                            tk.group([MM(ps[b2][:, :], pswap[:], qb.ap[:, u, :], True, True)], r=qb.k(u) + ["pswap"], w=PK(b2))
                            tk.op("dve", I("tensor_tensor", out=t2.ap[:, u, :], in0=ps[b2][:, :], in1=sin2[:], op=ALU.mult),
                                  r=PK(b2) + ["sin2"], w=t2.k(u))
                            tk.op("pool", I("tensor_tensor", out=dstT.ap[:, mi_, :], in0=t1.ap[:, u, :], in1=t2.ap[:, u, :], op=ALU.add),
                                  r=t1.k(u) + t2.k(u), w=dstT.k(mi_))
                        linear_fm(sl_m, rhs_h, hT_keys, KC, ev_m, mi_list=[h])
                        gcount += 1
                        if gcount in (1, 2, 3, 4):
                            conv_group(gcount - 1)
                tk.label = 'win_v'
                sl_v = ring_load(l, 6)
                for c in range(NCH):
                    b = nb()
                    tk.group([(MM(ps[b][:, :], hT[:, kc, c * 128:(c + 1) * 128], ring[sl_v][:, kc * 512:(kc + 1) * 512], kc == 0, kc == KC - 1), [hT_keys[kc]])
                              for kc in range(KC)], r=[("ring", sl_v)], w=PK(b))
                    tk.op("act", I("activation", out=vtok.ap[:, c, :], in_=ps[b][:, :], func=AF.Copy), r=PK(b), w=vtok.k(c))
                conv_tail_A()
                tk.label = 'ret'
                HS = [slice(h * 128, (h + 1) * 128) for h in range(H)]
                CS = [slice(c * 128, (c + 1) * 128) for c in range(NCH)]

                def retbuf(c):
                    return (retsb.ap[:, c, :], retsb.k(c)) if c < 2 else (th.ap[:, c - 2, :], th.k(c - 2))
                rn4 = t1.ap.bitcast(BF16).rearrange("p a (two b) -> p (a two) b", two=2)

                def rnk(c):
                    return t1.k(c // 2)

                def ret_TS(c):
                    u = c % 2
                    b = nb()
                    tk.group([TR(psb[b][:, HS[h]], kT.ap[:, h, CS[c]], identb[:]) for h in range(H)], r=kT.k() + ["identb"], w=PK(b))
                    tk.op("dve", I("tensor_tensor", out=kz.ap[:, u, :], in0=psb[b][:, 0:512], in1=con[:, C_ZTAB:C_ZTAB + 512], op=ALU.mult),
                          r=PK(b) + ["con"], w=kz.k(u))
                    b = nb()
                    tk.group([MM(ps[b][:, HS[h]], kT.ap[:, h, CS[c]], qT.ap[:, h, CS[c]], True, True) for h in range(H)], r=kT.k() + qT.k(), w=PK(b))
                    tk.op("dve", I("tensor_tensor", out=smk.ap[:, u, :], in0=ps[b][:, :], in1=con[:, C_DMASK:C_DMASK + 512], op=ALU.mult),
                          r=PK(b) + ["con"], w=smk.k(u))

                def ret_KIX(c, l=l):
                    u = c % 2
                    rb_ap, rb_k = retbuf(c)
                    bk = nb()
                    tk.group([MM(ps[bk][:, HS[h]], kz.ap[:, u, HS[h]], vtok.ap[:, c, HS[h]], True, True) for h in range(H)],
                             r=kz.k(u) + vtok.k(c), w=PK(bk))
                    bi = nb()
                    tk.group([MM(ps[bi][:, HS[h]], smk.ap[:, u, HS[h]], vtok.ap[:, c, HS[h]], True, True) for h in range(H)],
                             r=smk.k(u) + vtok.k(c), w=PK(bi))
                    bc = nb()
                    tk.group([MM(ps[bc][:, HS[h]], qT.ap[:, h, CS[c]], Sbf[:, l, HS[h]], True, True) for h in range(H)],
                             r=qT.k() + [f"Sbf{l}"], w=PK(bc))
                    for h in range(H):
                        tk.op("dve", I("scalar_tensor_tensor", out=Sst[:, l, HS[h]], in0=Sst[:, l, HS[h]], scalar=CD[h], in1=ps[bk][:, HS[h]],
                                       op0=ALU.mult, op1=ALU.add), r=PK(bk) + [f"S{l}"], w=[f"S{l}"])
                    tk.op("act", I("activation", out=Sbf[:, l, :], in_=Sst[:, l, :], func=AF.Copy), r=[f"S{l}"], w=[f"Sbf{l}"])
                    tk.op("act", I("activation", out=innsb.ap[:, u, :], in_=ps[bi][:, :], func=AF.Copy), r=PK(bi), w=innsb.k(u))
                    for h in range(H):
                        tk.op("dve", I("scalar_tensor_tensor", out=rb_ap[:, HS[h]], in0=ps[bc][:, HS[h]], scalar=con[:, C_XI + h:C_XI + h + 1],
                                       in1=innsb.ap[:, u, HS[h]], op0=ALU.mult, op1=ALU.add),
                              r=PK(bc) + innsb.k(u) + ["con"], w=rb_k)

                def ret_GN(c):
                    rb_ap, rb_k = retbuf(c)
                    for h in range(H):
                        tk.op("dve", I("bn_stats", out=stats[:, c, h, :], in_=rb_ap[:, HS[h]]), r=rb_k, w=[("stats", c, h)])
                        tk.op("dve", I("bn_aggr", out=mv[:, c, h, :], in_=stats[:, c, h, :]), r=[("stats", c, h)], w=[("mv", c)])
                    tk.op("dve", I("tensor_scalar", out=veps[:, c, :], in0=mv[:, c, :, 1], scalar1=LN_EPS, scalar2=None, op0=ALU.add),
                          r=[("mv", c)], w=[("veps", c)])
                    tk.op("pool", I("tensor_tensor", out=rstd4[:, c, :], in0=veps[:, c, :], in1=mhalf[:], op=ALU.pow),
                          r=[("veps", c), "mhalf"], w=[("rstd4", c)])
                    tk.op("pool", I("tensor_tensor", out=nmr[:, c, :], in0=mv[:, c, :, 0], in1=rstd4[:, c, :], op=ALU.mult),
                          r=[("mv", c), ("rstd4", c)], w=[("nmr", c)])
                    tk.op("pool", I("tensor_scalar", out=nmr[:, c, :], in0=nmr[:, c, :], scalar1=-1.0, scalar2=0.0, op0=ALU.mult, op1=ALU.add),
                          r=[("nmr", c)], w=[("nmr", c)])
                    for h in range(H):
                        tk.op("pool", I("tensor_scalar", out=rn4[:, c, HS[h]], in0=rb_ap[:, HS[h]], scalar1=rstd4[:, c, h:h + 1],
                                        scalar2=nmr[:, c, h:h + 1], op0=ALU.mult, op1=ALU.add),
                              r=rb_k + [("rstd4", c), ("nmr", c)], w=rnk(c))

                def ret_OUT(c, l=l):
                    u = c % 2
                    bt = nb()
                    tk.group([TR(psb[bt][:, HS[h]], rn4[:, c, HS[h]], identb[:]) for h in range(H)], r=rnk(c) + ["identb"], w=PK(bt))
                    for h in range(H):
                        tk.op("act", I("activation", out=gtmp.ap[:, u, HS[h]], in_=psb[bt][:, HS[h]], func=AF.Identity,
                                       scale=pc(l, "gng", h), bias=pc(l, "gnb", h)), r=PK(bt) + ["par"], w=gtmp.k(u))
                    tk.op("pool", I("tensor_tensor", out=mixT[:, 4:8, CS[c]], in0=gtmp.ap[:, u, :].rearrange("p (h q) -> p h q", h=4),
                                    in1=sgT.ap[:, :, CS[c]], op=ALU.mult),
                          r=gtmp.k(u) + sgT.k(), w=[("mixT", 4), ("mixT", 5), ("mixT", 6), ("mixT", 7)])

                ret_TS(0)
                ret_TS(1)
                ret_KIX(0)
                conv_tail_B()
                ret_TS(2)
                ret_KIX(1)
                conv_tail_C()
                ret_GN(0)
                ret_TS(3)
                ret_KIX(2)
                ret_GN(1)
                ret_KIX(3)
                conv_tail_D()
                ret_GN(2)
                ret_GN(3)
                tk.label = 'win_g'
                sl_g = ring_load(l, 7)

                def ev_g(mi_, b):
                    tk.op("act", I("activation", out=sgT.ap[:, mi_, :], in_=ps[b][:, :], func=AF.Silu), r=PK(b), w=sgT.k(mi_))
                linear_fm(sl_g, rhs_h, hT_keys, KC, ev_g)
                tk.label = 'ret'
                ret_OUT(0)
                ret_OUT(1)
                ret_OUT(2)
                ret_OUT(3)
                nxt = (ti, l + 1) if l + 1 < depth else (ti + 1, 0)
                if nxt[0] < NT and depth > 1:
                    emit_diag(nxt[1])
                tk.op("act", I("activation", out=tabw[:, 0:1], in_=tabw[:, 1:2], func=AF.Ln), r=[], w=["tabw"])

                if STAGE < 7:
                    continue
                tk.label = 'wout'
                mix_keys = [("mixT", kc) for kc in range(KC)]
                for half in range(2):
                    sl = ring_load(l, 8 + half)
                    linear_fm(sl, lambda kc: mixT[:, kc, :], mix_keys, KC, lambda mi_, b, half=half: residual_evac(half * 4 + mi_, b))

                if STAGE < 8:
                    continue
                if first:
                    emit_casts(gi + 10)
                tk.label = 'norm2'
                emit_norm(l, "gffn")
                if l == depth - 1 and ti + 1 < NT:
                    emit_tile_prefetch(ti + 1)
                tk.label = 'ff1'
                for pi in range(8):
                    sl = ring_load(l, 10 + pi)

                    def ev_ff1(mi_, b, pi=pi):
                        j = pi * 4 + mi_
                        u = j % 2
                        tk.op("act", I("activation", out=sqf.ap[:, u, :], in_=ps[b][:, :], func=AF.Square), r=PK(b), w=sqf.k(u))
                        tk.op("dve", I("scalar_tensor_tensor", out=hid.ap[:, j, :], in0=ps[b][:, :], scalar=0.0, in1=sqf.ap[:, u, :],
                                       op0=ALU.is_gt, op1=ALU.mult), r=PK(b) + sqf.k(u), w=hid.k(j))
                    if pi == 0:
                        linear_first([(sl, mi_) for mi_ in range(4)], rhs_h, hT_keys, KC, [ev_ff1] * 4)
                    else:
                        linear_fm(sl, rhs_h, hT_keys, KC, ev_ff1)
                tk.label = 'ff2'
                for half in range(2):
                    banks = [nb() for _ in range(4)]
                    for jb in range(4):
                        sl = ring_load(l, 18 + half * 4 + jb)
                        fns = []
                        for jj in range(8):
                            for mi in range(4):
                                fns.append((MM(ps[banks[mi]][:, :], ring[sl][:, jj * 512 + mi * 128: jj * 512 + mi * 128 + 128],
                                               hid.ap[:, jb * 8 + jj, :], jb == 0 and jj == 0, jb == 3 and jj == 7), hid.k(jb * 8 + jj)))
                        tk.group(fns, r=[("ring", sl)], w=[("ps", bk_) for bk_ in banks])
                    for mi in range(4):
                        residual_evac(half * 4 + mi, banks[mi])

                if STAGE < 9:
                    continue
                if first and l + 1 < depth:
                    emit_casts(gi + NG + 4)
                tk.label = 'ple'
                tk.dma("sp", I("dma_start", out=p_in.ap, in_=p_d[l, t0:t0 + T, :].rearrange("(c p) f -> p c f", p=128)), s_p, w=p_in.k())
                for k2 in range(2):
                    b = nb()
                    tk.group([TR(ps[b][:, c * 128:(c + 1) * 128], p_in.ap[:, c, k2 * 128:(k2 + 1) * 128], identf) for c in range(NCH)],
                             r=p_in.k() + ["con"], w=PK(b))
                    tk.op("act", I("activation", out=pT.ap[:, k2, :], in_=ps[b][:, :], func=AF.Copy, scale=0.5), r=PK(b), w=pT.k(k2))
                tk.label = 'norm3'
                emit_norm(l, "gple")
                tk.label = 'ple'
                sl_pp = ring_load(l, 28)

                def ple_tail(m, bg, sl_pp=sl_pp):
                    u = m % 2
                    tk.op("act", I("activation", out=thg.ap[:, u, :], in_=ps[bg][:, :], func=AF.Tanh, scale=0.5), r=PK(bg), w=thg.k(u))
                    b = nb()
                    tk.group([MM(ps[b][:, :], ring[sl_pp][:, k2 * 1024 + m * 128: k2 * 1024 + m * 128 + 128], pT.ap[:, k2, :], k2 == 0, k2 == 1)
                              for k2 in range(2)], r=[("ring", sl_pp)] + pT.k(), w=PK(b))
                    tk.op("dve", I("scalar_tensor_tensor", out=ps[b][:, :], in0=thg.ap[:, u, :], scalar=1.0, in1=ps[b][:, :],
                                   op0=ALU.add, op1=ALU.mult), r=thg.k(u) + PK(b), w=PK(b))
                    tk.op("dve", I("tensor_tensor", out=xT[:, m, :], in0=xT[:, m, :], in1=ps[b][:, :], op=ALU.add),
                          r=[("xT", m)] + PK(b), w=[("xT", m)])
                    tk.op("act", I("activation", out=sq[:, m, :], in_=xT[:, m, :], func=AF.Square), r=[("xT", m)], w=[("sq", m)])

                for half in range(2):
                    sl = ring_load(l, 26 + half)
                    if half == 0:
                        linear_first([(sl, mi_) for mi_ in range(4)], rhs_h, hT_keys, KC, [lambda mi_, bg: ple_tail(mi_, bg)] * 4)
                    else:
                        for mi in range(4):
                            linear_fm(sl, rhs_h, hT_keys, KC, lambda mi_, bg: ple_tail(4 + mi_, bg), mi_list=[mi])
                tk.op("act", I("activation", out=tabw[:, 0:1], in_=tabw[:, 1:2], func=AF.Ln), r=[], w=["tabw"])
                if first and l + 1 < depth:
                    emit_casts(gi + NG + 5)

            tk.label = 'final'
            if 'min0' in FLAGS or 'min1' in FLAGS:
                tk.dma("sp", I("dma_start", out=out_d[t0:t0 + T, :].rearrange("(c p) f -> p c f", p=128), in_=xstage.ap), s_o, r=xstage.k() + [("xT", m) for m in range(KC)] + [("sq", m) for m in range(KC)], w=["outd"])
                continue
            b = emit_rstd()
            if 'min2' in FLAGS:
                tk.dma("sp", I("dma_start", out=out_d[t0:t0 + T, :].rearrange("(c p) f -> p c f", p=128), in_=xstage.ap), s_o, r=xstage.k() + PK(b), w=["outd"])
                continue
            for m in range(KC):
                u = m % 2
                tk.op("dve", I("scalar_tensor_tensor", out=obuf.ap[:, u, :], in0=xT[:, m, :], scalar=par[:, 2 * PL + m:2 * PL + m + 1],
                               in1=ps[b][:, :], op0=ALU.mult, op1=ALU.mult), r=[("xT", m), "par"] + PK(b), w=obuf.k(u))
                bt = nb()
                tk.group([TR(ps[bt][:, c * 128:(c + 1) * 128], obuf.ap[:, u, c * 128:(c + 1) * 128], identf) for c in range(NCH)],
                         r=obuf.k(u) + ["con"], w=PK(bt))
                tk.op("act", I("activation", out=ostage.ap[:, :, m * 128:(m + 1) * 128], in_=ps[bt][:, :].rearrange("p (c f) -> p c f", c=4), func=AF.Copy),
                      r=PK(bt), w=ostage.k())
            oq = "sp" if "spout" in FLAGS else "act"
            tk.dma(oq, I("dma_start", out=out_d[t0:t0 + T, :].rearrange("(c p) f -> p c f", p=128), in_=ostage.ap), s_o, r=ostage.k(), w=["outd"])
        tk.final_wait("sp" if "spout" in FLAGS else "act", s_o)
        block = es.enter_context(nc.Block())
        tk.replay(block)
        if DUMP_LABELS:
            import json
            json.dump(tk.pe_labels, open(DUMP_LABELS, 'w'))
        print(f"[build] ops={tk.nops} waits={tk.nwait} sems={len(tk.sem)} banks_used={bank_ctr[0]}", flush=True)
    return nc


def host_consts():
    c = np.zeros((128, NCONST), np.float32)
    half = 64
    inv = (1.0 / (np.float32(10000.0) ** (np.arange(0, half, dtype=np.float32) * np.float32(2.0 / 128)))).astype(np.float32)
    c[:, C_INVF] = np.concatenate([inv, inv])
    c[:64, C_SIGN] = -1.0
    c[64:, C_SIGN] = 1.0
    idx = np.arange(128, dtype=np.float64)
    for h in range(H):
        lg = np.log(1.0 - 2.0 ** (-5.0 - h))
        c[:, C_XI + h] = np.exp((idx + 1.0) * lg)
        diff = idx[None, :] - idx[:, None]
        dm = np.where(diff >= 0, np.exp(np.maximum(diff, 0.0) * lg), 0.0) * (128.0 ** -0.5)
        c[:, C_DMASK + h * 128:C_DMASK + (h + 1) * 128] = dm
        zeta = np.exp((127.0 - idx) * lg) * (128.0 ** -0.5)
        c[:, C_ZTAB + h * 128:C_ZTAB + (h + 1) * 128] = zeta[:, None]
    c[:, C_IDENT:C_IDENT + 128] = np.eye(128, dtype=np.float32)
    for m in range(128):
        c[(m + 64) % 128, C_PSWAP + m] = 1.0
    return c


def host_params(inp):
    P = np.zeros((128, NPAR), np.float32)

    def cols(v, n):
        return np.asarray(v, np.float32).reshape(n, 128).T

    for l in range(DEPTH):
        b = l * PL
        P[:, b + 0:b + 8] = cols(inp["norm_mix_g"][l], 8)
        P[:, b + 8:b + 16] = cols(inp["norm_ffn_g"][l], 8)
        P[:, b + 16:b + 24] = cols(inp["norm_ple_g"][l], 8)
        P[:, b + 24:b + 28] = cols(inp["conv_dw_b"][l], 4)
        P[:, b + 28:b + 32] = cols(inp["conv_ln_g"][l], 4)
        P[:, b + 32:b + 36] = cols(inp["conv_ln_b"][l], 4)
        P[:, b + 36:b + 40] = cols(inp["ret_gn_g"][l], 4)
        P[:, b + 40:b + 44] = cols(inp["ret_gn_b"][l], 4)
        w = np.asarray(inp["conv_dw_w"][l], np.float32)
        P[:, b + 44:b + 168] = w.reshape(CW, 4, 128).transpose(2, 0, 1).reshape(128, CW * 4)
    P[:, 2 * PL:2 * PL + 8] = cols(inp["final_norm_g"], 8)
    return P


_NC_CACHE = {}


def run(inp, S, n_cores):
    if S not in _NC_CACHE:
        _NC_CACHE[S] = build(S)
    nc = _NC_CACHE[S]
    consts = host_consts()
    params = host_params(inp)
    f = lambda k: np.ascontiguousarray(np.asarray(inp[k], np.float32))
    shared = {"params": params, "consts": consts, "w_in": f("w_in"), "w_out": f("w_out"), "w_ff1": f("w_ff1"),
              "w_ff2": f("w_ff2"), "w_pg": f("w_ple_gate"), "w_pp": f("w_ple_proj")}
    x = np.asarray(inp["x"], np.float32)
    p = np.asarray(inp["p"], np.float32)
    pos = np.asarray(inp["positions"], np.int32)
    in_maps = []
    for b in range(n_cores):
        m = dict(shared)
        m["x"] = np.ascontiguousarray(x[b, :S])
        m["p"] = np.ascontiguousarray(p[:, b, :S])
        m["pos"] = np.ascontiguousarray(pos[b:b + 1, :S])
        in_maps.append(m)
    res = run_bass_kernel_spmd(nc, in_maps, core_ids=list(range(n_cores)))
    return np.stack([r["out"] for r in res.results], axis=0)


def kernel(**inputs):
    return run(inputs, SEQ, BATCH).astype(np.float32)
```

```python
import math
from contextlib import ExitStack

import numpy as np
import concourse.bass as bass
import concourse.mybir as mybir
from concourse.bass_utils import run_bass_kernel_spmd

F32 = mybir.dt.float32
BF16 = mybir.dt.bfloat16
I32 = mybir.dt.int32
AF = mybir.ActivationFunctionType
ALU = mybir.AluOpType

D = 1024
KC = 8
T = 512
NCH = T // 128
H = 4
CW = 31
HALO = CW - 1
PLE = 256
DFF = 4096
NPIECE = 29
SEQ = 8192
BATCH = 8
DEPTH = 2
RMS_EPS = 1e-6
LN_EPS = 1e-5
PL = 168
NPAR = 2 * PL + 8
C_INVF, C_SIGN, C_XI, C_DMASK, C_ZTAB, C_IDENT = 0, 1, 2, 6, 6 + 512, 6 + 1024
C_PSWAP = 6 + 1024 + 128
NCONST = 6 + 1024 + 256
PI = math.pi
TWO_PI = 2.0 * math.pi


class Tracker:
    ENG = ("pe", "act", "dve", "pool", "sp")

    def __init__(self, nc, es):
        self.nc = nc
        self.es = es
        self.streams = {e: [] for e in self.ENG}
        self.sem = {}
        self.cnt = {}
        self.waited = {e: {} for e in self.ENG}
        self.state = {}
        self.engsem = {}
        for e in ("pe", "act", "dve", "pool"):
            self.engsem[e] = self.new_sem("e_" + e)
        self.engsem["sp"] = None
        self.nwait = 0
        self.nops = 0
        self.label = ''
        self.pe_labels = []

    def new_sem(self, name):
        self.sem[name] = self.es.enter_context(self.nc.semaphore(name))
        self.cnt[name] = 0
        return name

    def _waits(self, eng, r, w):
        raw, oth = {}, {}

        def add(d, sv):
            s, v = sv
            if d.get(s, 0) < v:
                d[s] = v

        for k in r:
            st = self.state.get(k)
            if st and st[0]:
                add(raw, st[0])
            if st and isinstance(k, tuple) and k[0] == "ps":
                for s, v in st[1].items():
                    add(oth, (s, v))
        for k in w:
            st = self.state.get(k)
            if st:
                if st[0]:
                    add(oth, st[0])
                for s, v in st[1].items():
                    add(oth, (s, v))
        own = self.engsem[eng]
        need = dict(raw)
        for s, v in oth.items():
            if s == own:
                continue
            if need.get(s, 0) < v:
                need[s] = v
        wd = self.waited[eng]
        for s, v in need.items():
            if wd.get(s, 0) >= v:
                continue
            wd[s] = v
            self.streams[eng].append(("w", s, v))
            self.nwait += 1

    def _commit(self, r, w, sem, val):
        for k in w:
            self.state[k] = [(sem, val), {}]
        for k in r:
            st = self.state.get(k)
            if st is None:
                st = self.state[k] = [None, {}]
            st[1][sem] = val

    def op(self, eng, fn, r=(), w=()):
        self._waits(eng, r, w)
        sem = self.engsem[eng]
        self.cnt[sem] += 1
        self.streams[eng].append(("o", fn, sem, 1))
        self._commit(r, w, sem, self.cnt[sem])
        self.nops += 1

    def group(self, fns, r=(), w=()):
        self._waits("pe", r, w)
        sem = self.engsem["pe"]
        self.cnt[sem] += 1
        self.pe_labels.extend([self.label] * len(fns))
        allr = list(r)
        n = len(fns)
        for i, f in enumerate(fns):
            if isinstance(f, tuple):
                f, ri = f
                self._waits("pe", ri, ())
                allr.extend(ri)
            if i < n - 1:
                self.streams["pe"].append(("o", f, None, 0))
            else:
                self.streams["pe"].append(("o", f, sem, 1))
        self._commit(allr, w, sem, self.cnt[sem])
        self.nops += n

    def dma(self, eng, fn, sem, r=(), w=()):
        self._waits(eng, r, w)
        self.cnt[sem] += 16
        self.streams[eng].append(("o", fn, sem, 16))
        self._commit(r, w, sem, self.cnt[sem])

    def final_wait(self, eng, sem):
        self.streams[eng].append(("w", sem, self.cnt[sem]))

    def replay(self, block):
        def run(name):
            def f(e):
                for it in self.streams[name]:
                    if it[0] == "w":
                        e.wait_ge(self.sem[it[1]], it[2])
                    else:
                        ins = it[1](e)
                        if it[2] is not None:
                            ins.then_inc(self.sem[it[2]], it[3])
            return f

        block.tensor(run("pe"))
        block.scalar(run("act"))
        block.vector(run("dve"))
        block.gpsimd(run("pool"))
        block.sync(run("sp"))


def _pieces_src(W, l, pi):
    w_in, w_out, w_ff1, w_ff2, w_pg, w_pp = W

    def std(mat, r0, r1, c0):
        return mat[l, r0:r1, c0:c0 + 512].rearrange("(kc p) j -> p kc j", p=128)

    if pi < 8:
        cols = {0: 0, 1: 512, 2: 1024, 3: 1024, 4: 1536, 5: 1536, 6: 2048, 7: 2560}[pi]
        if pi in (3, 5):
            v = w_in[l, :, cols:cols + 512].rearrange("(kc p) (h two d) -> p kc h two d", p=128, two=2, d=64)
            return [("swap", (kc, 1 - tw), v[:, kc, :, tw, :]) for kc in range(8) for tw in range(2)]
        return [("std", None, std(w_in, 0, D, cols))]
    if pi < 10:
        return [("std", None, std(w_out, 0, D, (pi - 8) * 512))]
    if pi < 18:
        return [("std", None, std(w_ff1, 0, D, (pi - 10) * 512))]
    if pi < 26:
        half, jb = divmod(pi - 18, 4)
        return [("std", None, std(w_ff2, jb * 1024, (jb + 1) * 1024, half * 512))]
    if pi < 28:
        return [("std", None, std(w_pg, 0, D, (pi - 26) * 512))]
    return [("proj", None, w_pp[l].rearrange("(kc p) j -> p kc j", p=128))]


CAST_GROUPS = [[1, 0], [2], [4], [6, 7], [8, 9], [10, 11, 12, 13], [14, 15, 16, 17],
               [18, 19, 20, 21], [22, 23, 24, 25], [26, 27, 28]]


def I(method, *a, **kw):
    return lambda e: getattr(e, method)(*a, **kw)


STAGE = 99
DUMP_LABELS = None
FLAGS = set()


def build(S, depth=DEPTH):
    NT = S // T
    nc = bass.Bass("TRN2", target_bir_lowering=False)
    dt = nc.dram_tensor
    x_d = dt("x", [S, D], F32, kind="ExternalInput").ap()
    p_d = dt("p", [DEPTH, S, PLE], F32, kind="ExternalInput").ap()
    pos_d = dt("pos", [1, S], I32, kind="ExternalInput").ap()
    par_d = dt("params", [128, NPAR], F32, kind="ExternalInput").ap()
    con_d = dt("consts", [128, NCONST], F32, kind="ExternalInput").ap()
    W = (dt("w_in", [DEPTH, D, 3072], F32, kind="ExternalInput").ap(),
         dt("w_out", [DEPTH, D, D], F32, kind="ExternalInput").ap(),
         dt("w_ff1", [DEPTH, D, DFF], F32, kind="ExternalInput").ap(),
         dt("w_ff2", [DEPTH, DFF, D], F32, kind="ExternalInput").ap(),
         dt("w_pg", [DEPTH, D, D], F32, kind="ExternalInput").ap(),
         dt("w_pp", [DEPTH, PLE, D], F32, kind="ExternalInput").ap())
    out_d = dt("out", [S, D], F32, kind="ExternalOutput").ap()
    wscr = dt("wscr", [DEPTH, NPIECE, 128, 4096], BF16, kind="Internal").ap()

    es = ExitStack()
    with es:
        tk = Tracker(nc, es)
        sb = lambda name, shape, d: es.enter_context(nc.sbuf_tensor(name, shape, d))
        xT = sb("xT", [128, KC, T], F32)
        sq = sb("sq", [128, KC, T], BF16)
        hT = sb("hT", [128, KC, T], BF16)
        mixT = sb("mixT", [128, KC, T], BF16)
        Sst = sb("Sst", [128, DEPTH, 512], F32)
        Sbf = sb("Sbf", [128, DEPTH, 512], BF16)
        diag = sb("diag", [128, CW * 4, 128], BF16)
        cos2 = sb("cos2", [128, T], F32)
        sin2 = sb("sin2", [128, T], F32)
        NSLOT = 4
        ring = [sb(f"ring{i}", [128, 4096], BF16) for i in range(NSLOT)]
        par = sb("par", [128, NPAR], F32)
        con = sb("con", [128, NCONST], F32)
        whalf = sb("whalf", [128, DEPTH, CW * 4], F32)
        identb = sb("identb", [128, 128], BF16)
        ones1k = sb("ones1k", [128, 128], BF16)
        ones512 = sb("ones512", [128, 128], BF16)
        halo = sb("halo", [128, DEPTH, 4, 32], BF16)
        stats = sb("stats", [128, 4, 4, 6], F32)
        mv = sb("mv", [128, 4, 4, 2], F32)
        rstd4 = sb("rstd4", [128, 4, 4], F32)
        veps = sb("veps", [128, 4, 4], F32)
        nmr = sb("nmr", [128, 4, 4], F32)
        mhalf = sb("mhalf", [128, 4], F32)
        sdn = sb("sdn", [128, T], F32)
        tabw = sb("tabw", [128, 2], F32)
        pswap = sb("pswap", [128, 128], BF16)
        epsr = sb("epsr", [128, 2], F32)
        uoff = [0]
        U_BYTES = 81 * 1024
        U = sb("U", [128, U_BYTES // 4], F32)

        class UB:
            def __init__(s, nbytes, dtype, shape3=None, phase_off=None):
                if phase_off is not None:
                    uoff[0] = phase_off
                s.off = uoff[0]
                s.nbytes = nbytes
                uoff[0] += (nbytes + 1023) // 1024 * 1024
                assert uoff[0] <= U_BYTES, (uoff[0], U_BYTES)
                v = U[:, s.off // 4:(s.off + nbytes) // 4]
                if dtype != F32:
                    v = v.bitcast(dtype)
                if shape3 is not None:
                    v = v.rearrange("p (a b) -> p a b", a=shape3[0])
                    s.nch = shape3[0]
                else:
                    s.nch = 1
                s.ap = v

            def k(s, i=None, n=1):
                if i is None:
                    lo, hi = s.off, s.off + s.nbytes
                else:
                    cb = s.nbytes // s.nch
                    lo, hi = s.off + i * cb, s.off + (i + n) * cb
                return [("U", g) for g in range(lo // 1024, (hi + 1023) // 1024)]

        class PB:
            def __init__(s, name, shape, dtype):
                s.t = sb(name, shape, dtype)
                s.ap = s.t
                s.name = name

            def k(s, i=None, n=1):
                return [(s.name, i)] if i is not None else [(s.name, j) for j in range(s.t.shape[1])]
        qb = PB("qb", [128, 2, 512], BF16)
        hglu = UB(4 * 544 * 2, BF16, (4, 544), phase_off=0)
        th = UB(2 * 512 * 4, F32, (2, 512))
        qT = UB(4 * 512 * 2, BF16, (4, 512))
        kT = UB(4 * 512 * 2, BF16, (4, 512))
        t1 = UB(2 * 512 * 4, F32, (2, 512))
        t2 = UB(2 * 512 * 4, F32, (2, 512))
        vtok = UB(4 * 512 * 2, BF16, (4, 512))
        sgT = UB(4 * 512 * 4, F32, (4, 512))
        kz = UB(2 * 512 * 2, BF16, (2, 512))
        smk = UB(2 * 512 * 2, BF16, (2, 512))
        innsb = UB(2 * 512 * 4, F32, (2, 512))
        retsb = UB(2 * 512 * 4, F32, (2, 512))
        rn = UB(2 * 512 * 2, BF16, (2, 512))
        gtmp = UB(2 * 512 * 4, F32, (2, 512))
        ycv = UB(4 * 512 * 4, F32, (4, 512))
        ybf = UB(4 * 512 * 2, BF16, (4, 512))
        ysq = UB(4 * 512 * 2, BF16, (4, 512))
        ytmp = UB(2 * 512 * 4, F32, (2, 512))
        msb = UB(512 * 4, F32)
        varb = UB(512 * 4, F32)
        sdb = UB(512 * 4, F32)
        hid = UB(32 * 512 * 2, BF16, (32, 512), phase_off=0)
        sqf = UB(2 * 512 * 4, F32, (2, 512))
        thg = UB(2 * 512 * 4, F32, (2, 512))
        pltmp = UB(2 * 512 * 4, F32, (2, 512))
        p_in = UB(4 * 256 * 4, F32, (4, 256))
        pT = UB(2 * 512 * 2, BF16, (2, 512))
        xstage = UB(4 * 1024 * 4, F32, (4, 1024), phase_off=50 * 1024)
        posi = UB(512 * 4, I32)
        ang = UB(512 * 4, F32)
        kfb = UB(512 * 4, F32)
        kib = UB(512 * 4, I32)
        rr = UB(512 * 4, F32)
        wa = UB(512 * 4, F32)
        wb_ = UB(512 * 4, F32)
        ostage = UB(4 * 1024 * 4, F32, (4, 1024), phase_off=0)
        obuf = UB(2 * 512 * 4, F32, (2, 512))

        ps = [es.enter_context(nc.psum_tensor(f"ps{i}", [128, 512], F32)) for i in range(8)]
        psb = [p[:, :].bitcast(BF16) for p in ps]
        bank_ctr = [0]

        def nb():
            b = bank_ctr[0] % 8
            bank_ctr[0] += 1
            return b

        PK = lambda b: [("ps", b)]
        s_const = tk.new_sem("d_const")
        s_ring = [tk.new_sem(f"d_ring{i}") for i in range(NSLOT)]
        s_x = tk.new_sem("d_x")
        s_o = tk.new_sem("d_o")
        s_p = tk.new_sem("d_p")
        s_pos = tk.new_sem("d_pos")
        NG = len(CAST_GROUPS)
        NCS = 4
        s_cs = [tk.new_sem(f"d_cs{i}") for i in range(NCS)]
        piece_keys = {}

        def MM(out, lhsT, rhs, start, stop):
            return lambda e: e.matmul(out, lhsT=lhsT, rhs=rhs, start=start, stop=stop)

        def TR(out, in_, ident):
            return lambda e: e.transpose(out=out, in_=in_, identity=ident)

        tk.dma("sp", I("dma_start", out=par[:], in_=par_d), s_const, w=["par"])
        tk.dma("sp", I("dma_start", out=con[:], in_=con_d), s_const, w=["con"])
        fin = (s_const, tk.cnt[s_const])
        tk.state["par"] = [fin, {}]
        tk.state["con"] = [fin, {}]
        identf = con[:, C_IDENT:C_IDENT + 128]
        tk.op("dve", I("tensor_copy", out=identb[:], in_=identf), r=["con"], w=["identb"])
        tk.op("dve", I("tensor_copy", out=pswap[:], in_=con[:, C_PSWAP:C_PSWAP + 128]), r=["con"], w=["pswap"])
        tk.op("dve", I("memset", ones1k[:], 1.0 / 1024.0), w=["ones1k"])
        tk.op("dve", I("memset", ones512[:], 1.0 / 512.0), w=["ones512"])
        tk.op("dve", I("memset", Sst[:], 0.0), w=["S0", "S1"])
        tk.op("dve", I("memset", Sbf[:], 0.0), w=["Sbf0", "Sbf1"])
        tk.op("dve", I("memset", halo[:], 0.0), w=["halo0", "halo1"])
        tk.op("dve", I("memset", mhalf[:], -0.5), w=["mhalf"])
        tk.op("dve", I("memset", tabw[:], 1.0), w=["tabw"])
        tk.op("dve", I("memset", epsr[:, 0:1], RMS_EPS), w=["epsr"])
        tk.op("dve", I("memset", epsr[:, 1:2], LN_EPS), w=["epsr"])
        for l in range(depth):
            tk.op("dve", I("tensor_scalar", out=whalf[:, l, :], in0=par[:, l * PL + 44:l * PL + 168],
                           scalar1=0.5, scalar2=None, op0=ALU.mult), r=["par"], w=[("whalf", l)])

        cast_ptr = [0]
        cast_list = [(l, g) for l in range(depth) for g in range(NG)]

        cast_dma_ctr = [0]

        def emit_casts(upto):
            if 'nocast' in FLAGS:
                return
            while cast_ptr[0] < min(upto, len(cast_list)):
                l, g = cast_list[cast_ptr[0]]
                cast_ptr[0] += 1
                for pi in CAST_GROUPS[g]:
                    dst = wscr[l, pi]
                    deps = []
                    for kind, sw, src in _pieces_src(W, l, pi):
                        if kind == "std":
                            dv = dst.rearrange("p (kc j) -> p kc j", kc=8)
                        elif kind == "proj":
                            dv = dst[:, 0:2048].rearrange("p (kc j) -> p kc j", kc=2)
                        else:
                            dv = dst.rearrange("p (kc h two d) -> p kc h two d", kc=8, h=4, two=2)[:, sw[0], :, sw[1], :]
                        sl_ = cast_dma_ctr[0] % NCS
                        cast_dma_ctr[0] += 1
                        sem = s_cs[sl_]
                        key = ("wscr", l, pi, len(deps))
                        tk.dma("pool", I("dma_start", out=dv, in_=src), sem, r=[], w=[("castslot", sl_), key])
                        deps.append(key)
                    piece_keys[(l, pi)] = deps

        piece_group = {pi: g for g, lst in enumerate(CAST_GROUPS) for pi in lst}
        ring_ctr = [0]

        def ring_load(l, pi):
            emit_casts(l * NG + piece_group[pi] + 1)
            s = ring_ctr[0] % NSLOT
            ring_ctr[0] += 1
            n = 2048 if pi == 28 else 4096
            tk.dma("sp", I("dma_start", out=ring[s][:, 0:n], in_=wscr[l, pi][:, 0:n]), s_ring[s],
                   r=piece_keys[(l, pi)], w=[("ring", s)])
            return s

        def pc(l, name, n):
            base = {"gmix": 0, "gffn": 8, "gple": 16, "dwb": 24, "lng": 28, "lnb": 32, "gng": 36, "gnb": 40}[name]
            c = l * PL + base + n
            return par[:, c:c + 1]

        def emit_diag(l):
            if 'nodiag' in FLAGS:
                return
            for j in range(CW):
                for cc in range(4):
                    idx = j * 4 + cc
                    tk.op("pool", I("tensor_scalar", out=diag[:, idx, :], in0=identb[:], scalar1=whalf[:, l, idx:idx + 1],
                                    scalar2=0.0, op0=ALU.mult, op1=ALU.add),
                          r=["identb", ("whalf", l)], w=[("diag", idx)])

        def emit_rstd():
            b = nb()
            tk.group([(MM(ps[b][:, :], ones1k[:], sq[:, kc, :], kc == 0, kc == KC - 1), [("sq", kc)]) for kc in range(KC)],
                     r=["ones1k"], w=PK(b))
            tk.op("act", I("activation", out=sdn[:], in_=ps[b][:, :], func=AF.Ln, bias=epsr[:, 0:1], scale=1.0), r=PK(b) + ["epsr"], w=["sdn"])
            tk.op("act", I("activation", out=ps[b][:, :], in_=sdn[:], func=AF.Exp, scale=-0.5), r=["sdn"], w=PK(b))
            return b

        def emit_norm(l, gname):
            b = emit_rstd()
            for kc in range(KC):
                tk.op("dve", I("scalar_tensor_tensor", out=hT[:, kc, :], in0=xT[:, kc, :], scalar=pc(l, gname, kc),
                               in1=ps[b][:, :], op0=ALU.mult, op1=ALU.mult),
                      r=[("xT", kc), "par"] + PK(b), w=[("hT", kc)])

        def linear_fm(slot, rhs_fn, rhs_keys, nk, evac, mi_list=range(4)):
            for mi in mi_list:
                b = nb()
                tk.group([(MM(ps[b][:, :], ring[slot][:, kc * 512 + mi * 128: kc * 512 + mi * 128 + 128], rhs_fn(kc), kc == 0, kc == nk - 1), [rhs_keys[kc]])
                          for kc in range(nk)], r=[("ring", slot)], w=PK(b))
                evac(mi, b)

        def linear_first(pairs, rhs_fn, rhs_keys, nk, evacs):
            banks = [nb() for _ in pairs]
            fns = []
            for kc in range(nk):
                for (slot, mi), b in zip(pairs, banks):
                    fns.append((MM(ps[b][:, :], ring[slot][:, kc * 512 + mi * 128: kc * 512 + mi * 128 + 128], rhs_fn(kc), kc == 0, kc == nk - 1),
                                [rhs_keys[kc]]))
            tk.group(fns, r=[("ring", s) for s in sorted({p[0] for p in pairs})], w=[("ps", b) for b in banks])
            for ev, ((slot, mi), b) in zip(evacs, zip(pairs, banks)):
                ev(mi, b)

        def residual_evac(m, b):
            tk.op("dve", I("tensor_tensor", out=xT[:, m, :], in0=xT[:, m, :], in1=ps[b][:, :], op=ALU.add),
                  r=[("xT", m)] + PK(b), w=[("xT", m)])
            tk.op("act", I("activation", out=sq[:, m, :], in_=xT[:, m, :], func=AF.Square), r=[("xT", m)], w=[("sq", m)])

        hT_keys = [("hT", kc) for kc in range(KC)]
        rhs_h = lambda kc: hT[:, kc, :]
        CD = [float(np.exp(128.0 * np.log(1.0 - 2.0 ** (-5.0 - h)))) for h in range(H)]
        C1 = 6.28125
        C2 = TWO_PI - C1

        def emit_tile_prefetch(ti):
            t0 = ti * T
            tk.dma("sp", I("dma_start", out=posi.ap, in_=pos_d[0:1, t0:t0 + T].partition_broadcast(128)), s_pos, w=posi.k())
            tk.op("dve", I("tensor_copy", out=ang.ap, in_=posi.ap), r=posi.k(), w=ang.k())
            tk.op("dve", I("tensor_scalar", out=ang.ap, in0=ang.ap, scalar1=con[:, C_INVF:C_INVF + 1], scalar2=None, op0=ALU.mult),
                  r=ang.k() + ["con"], w=ang.k())
            tk.op("dve", I("tensor_scalar", out=kfb.ap, in0=ang.ap, scalar1=1.0 / TWO_PI, scalar2=None, op0=ALU.mult), r=ang.k(), w=kfb.k())
            tk.op("dve", I("tensor_copy", out=kib.ap, in_=kfb.ap), r=kfb.k(), w=kib.k())
            tk.op("dve", I("tensor_copy", out=kfb.ap, in_=kib.ap), r=kib.k(), w=kfb.k())
            tk.op("dve", I("scalar_tensor_tensor", out=rr.ap, in0=kfb.ap, scalar=-C1, in1=ang.ap, op0=ALU.mult, op1=ALU.add),
                  r=kfb.k() + ang.k(), w=rr.k())
            tk.op("dve", I("scalar_tensor_tensor", out=rr.ap, in0=kfb.ap, scalar=-C2, in1=rr.ap, op0=ALU.mult, op1=ALU.add),
                  r=kfb.k() + rr.k(), w=rr.k())
            for (shift, dst, key, scl) in ((PI / 2, cos2, "cos2", None), (0.0, sin2, "sin2", con[:, C_SIGN:C_SIGN + 1])):
                tk.op("dve", I("tensor_scalar", out=wa.ap, in0=rr.ap, scalar1=shift, scalar2=None, op0=ALU.add), r=rr.k(), w=wa.k())
                tk.op("dve", I("tensor_scalar", out=wb_.ap, in0=wa.ap, scalar1=PI, scalar2=TWO_PI, op0=ALU.is_gt, op1=ALU.mult), r=wa.k(), w=wb_.k())
                tk.op("dve", I("tensor_tensor", out=wa.ap, in0=wa.ap, in1=wb_.ap, op=ALU.subtract), r=wa.k() + wb_.k(), w=wa.k())
                tk.op("dve", I("tensor_scalar", out=wb_.ap, in0=wa.ap, scalar1=-PI, scalar2=TWO_PI, op0=ALU.is_lt, op1=ALU.mult), r=wa.k(), w=wb_.k())
                tk.op("dve", I("tensor_tensor", out=wa.ap, in0=wa.ap, in1=wb_.ap, op=ALU.add), r=wa.k() + wb_.k(), w=wa.k())
                tk.op("dve", I("tensor_scalar", out=wa.ap, in0=wa.ap, scalar1=3.1415925, scalar2=-3.1415925, op0=ALU.min, op1=ALU.max), r=wa.k(), w=wa.k())
                if scl is None:
                    tk.op("act", I("activation", out=dst[:], in_=wa.ap, func=AF.Sin), r=wa.k(), w=[key])
                else:
                    tk.op("act", I("activation", out=dst[:], in_=wa.ap, func=AF.Sin, scale=scl), r=wa.k() + ["con"], w=[key])
            tk.op("act", I("activation", out=tabw[:, 0:1], in_=tabw[:, 1:2], func=AF.Ln), r=[], w=["tabw"])
            tk.dma("sp", I("dma_start", out=xstage.ap, in_=x_d[t0:t0 + T, :].rearrange("(c p) f -> p c f", p=128)), s_x, w=xstage.k())

        emit_casts(4)
        emit_diag(0)
        emit_tile_prefetch(0)
        for ti in range(NT):
            t0 = ti * T
            for m in range(KC if 'min0' not in FLAGS else 0):
                b = nb()
                tk.group([TR(ps[b][:, c * 128:(c + 1) * 128], xstage.ap[:, c, m * 128:(m + 1) * 128], identf) for c in range(NCH)],
                         r=xstage.k() + ["con"], w=PK(b))
                tk.op("dve", I("tensor_copy", out=xT[:, m, :], in_=ps[b][:, :]), r=PK(b), w=[("xT", m)])
                tk.op("act", I("activation", out=sq[:, m, :], in_=ps[b][:, :], func=AF.Square), r=PK(b), w=[("sq", m)])

            for l in range(depth if STAGE >= 2 else 0):
                first = (ti == 0)
                gi = l * NG
                if first:
                    emit_casts(gi + 5)
                tk.label = 'norm1'
                emit_norm(l, "gmix")
                tk.op("pool", I("tensor_copy", out=hglu.ap[:, :, 0:HALO], in_=halo[:, l, :, 0:HALO]), r=[f"halo{l}"], w=hglu.k())
                tk.label = 'win_ab'
                sl_b = ring_load(l, 1)
                sl_a = ring_load(l, 0)
                for mi in range(4):
                    def ev_b(mi_, b):
                        tk.op("act", I("activation", out=th.ap[:, mi_ % 2, :], in_=ps[b][:, :], func=AF.Tanh, scale=0.5), r=PK(b), w=th.k(mi_ % 2))

                    def ev_a(mi_, b):
                        tk.op("dve", I("scalar_tensor_tensor", out=hglu.ap[:, mi_, HALO:HALO + T], in0=th.ap[:, mi_ % 2, :], scalar=1.0,
                                       in1=ps[b][:, :], op0=ALU.add, op1=ALU.mult), r=th.k(mi_ % 2) + PK(b), w=hglu.k(mi_))
                    if mi == 0:
                        linear_first([(sl_b, 0), (sl_a, 0), (sl_b, 1), (sl_a, 1)], rhs_h, hT_keys, KC, [ev_b, ev_a, ev_b, ev_a])
                    elif mi >= 2:
                        linear_fm(sl_b, rhs_h, hT_keys, KC, ev_b, mi_list=[mi])
                        linear_fm(sl_a, rhs_h, hT_keys, KC, ev_a, mi_list=[mi])
                tk.op("pool", I("tensor_copy", out=halo[:, l, :, 0:HALO], in_=hglu.ap[:, :, T:T + HALO]), r=hglu.k(), w=[f"halo{l}"])
                def conv_group(cc, l=l):
                    tk.label = 'conv'
                    b = nb()
                    tk.group([MM(ps[b][:, :], diag[:, j * 4 + cc, :], hglu.ap[:, cc, j:j + T], j == 0, j == CW - 1) for j in range(CW)],
                             r=[("diag", j * 4 + cc) for j in range(CW)] + hglu.k(cc), w=PK(b))
                    tk.op("act", I("activation", out=ycv.ap[:, cc, :], in_=ps[b][:, :], func=AF.Identity, bias=pc(l, "dwb", cc), scale=1.0),
                          r=PK(b) + ["par"], w=ycv.k(cc))
                    tk.op("act", I("activation", out=ybf.ap[:, cc, :], in_=ps[b][:, :], func=AF.Identity, bias=pc(l, "dwb", cc), scale=1.0),
                          r=PK(b) + ["par"], w=ybf.k(cc))
                    tk.op("act", I("activation", out=ysq.ap[:, cc, :], in_=ycv.ap[:, cc, :], func=AF.Square), r=ycv.k(cc), w=ysq.k(cc))

                def conv_tail_A():
                    tk.label = 'convstat'
                    bm = nb()
                    tk.group([(MM(ps[bm][:, :], ones512[:], ybf.ap[:, cc, :], cc == 0, cc == 3), ybf.k(cc)) for cc in range(4)], r=["ones512"], w=PK(bm))
                    be = nb()
                    tk.group([(MM(ps[be][:, :], ones512[:], ysq.ap[:, cc, :], cc == 0, cc == 3), ysq.k(cc)) for cc in range(4)], r=["ones512"], w=PK(be))
                    tk.op("act", I("activation", out=msb.ap, in_=ps[bm][:, :], func=AF.Copy), r=PK(bm), w=msb.k())
                    tk.op("act", I("activation", out=varb.ap, in_=ps[be][:, :], func=AF.Copy), r=PK(be), w=varb.k())
                    tk.op("pool", I("tensor_tensor", out=sdb.ap, in0=msb.ap, in1=msb.ap, op=ALU.mult), r=msb.k(), w=sdb.k())
                    tk.op("pool", I("tensor_tensor", out=varb.ap, in0=varb.ap, in1=sdb.ap, op=ALU.subtract), r=varb.k() + sdb.k(), w=varb.k())

                def conv_tail_B():
                    tk.op("act", I("activation", out=sdb.ap, in_=varb.ap, func=AF.Ln, bias=epsr[:, 1:2], scale=1.0), r=varb.k() + ["epsr"], w=sdb.k())
                    tk.op("act", I("activation", out=sdb.ap, in_=sdb.ap, func=AF.Exp, scale=-0.5), r=sdb.k(), w=sdb.k())

                def conv_tail_C():
                    for cc in range(4):
                        tk.op("pool", I("tensor_tensor", out=ycv.ap[:, cc, :], in0=ycv.ap[:, cc, :], in1=msb.ap, op=ALU.subtract),
                              r=ycv.k(cc) + msb.k(), w=ycv.k(cc))
                        tk.op("pool", I("tensor_tensor", out=ycv.ap[:, cc, :], in0=ycv.ap[:, cc, :], in1=sdb.ap, op=ALU.mult),
                              r=ycv.k(cc) + sdb.k(), w=ycv.k(cc))

                def conv_tail_D(l=l):
                    for cc in range(4):
                        tk.op("act", I("activation", out=mixT[:, cc, :], in_=ycv.ap[:, cc, :], func=AF.Silu, scale=pc(l, "lng", cc), bias=pc(l, "lnb", cc)),
                              r=ycv.k(cc) + ["par"], w=[("mixT", cc)])

                if first:
                    emit_casts(gi + 8)
                gcount = 0
                for (p_main, p_sw, dstT) in ((2, 3, qT), (4, 5, kT)):
                    sl_m = ring_load(l, p_main)
                    for h in range(H):
                        tk.label = 'win_qk'

                        def ev_m(mi_, b, dstT=dstT):
                            u = mi_ % 2
                            tk.op("act", I("activation", out=qb.ap[:, u, :], in_=ps[b][:, :], func=AF.Copy), r=PK(b), w=qb.k(u))
                            tk.op("dve", I("tensor_tensor", out=t1.ap[:, u, :], in0=ps[b][:, :], in1=cos2[:], op=ALU.mult),
                                  r=PK(b) + ["cos2"], w=t1.k(u))
                            b2 = nb()
                            tk.group([MM(ps[b2][:, :], pswap[:], qb.ap[:, u, :], True, True)], r=qb.k(u) + ["pswap"], w=PK(b2))
                            tk.op("dve", I("tensor_tensor", out=t2.ap[:, u, :], in0=ps[b2][:, :], in1=sin2[:], op=ALU.mult),
                                  r=PK(b2) + ["sin2"], w=t2.k(u))
                            tk.op("pool", I("tensor_tensor", out=dstT.ap[:, mi_, :], in0=t1.ap[:, u, :], in1=t2.ap[:, u, :], op=ALU.add),
                                  r=t1.k(u) + t2.k(u), w=dstT.k(mi_))
                        linear_fm(sl_m, rhs_h, hT_keys, KC, ev_m, mi_list=[h])
                        gcount += 1
                        if gcount in (1, 2, 3, 4):
                            conv_group(gcount - 1)
                tk.label = 'win_v'
                sl_v = ring_load(l, 6)
                for c in range(NCH):
                    b = nb()
                    tk.group([(MM(ps[b][:, :], hT[:, kc, c * 128:(c + 1) * 128], ring[sl_v][:, kc * 512:(kc + 1) * 512], kc == 0, kc == KC - 1), [hT_keys[kc]])
                              for kc in range(KC)], r=[("ring", sl_v)], w=PK(b))
                    tk.op("act", I("activation", out=vtok.ap[:, c, :], in_=ps[b][:, :], func=AF.Copy), r=PK(b), w=vtok.k(c))
                conv_tail_A()
                tk.label = 'ret'
                HS = [slice(h * 128, (h + 1) * 128) for h in range(H)]
                CS = [slice(c * 128, (c + 1) * 128) for c in range(NCH)]

                def retbuf(c):
                    return (retsb.ap[:, c, :], retsb.k(c)) if c < 2 else (th.ap[:, c - 2, :], th.k(c - 2))
                rn4 = t1.ap.bitcast(BF16).rearrange("p a (two b) -> p (a two) b", two=2)

                def rnk(c):
                    return t1.k(c // 2)

                def ret_TS(c):
                    u = c % 2
                    b = nb()
                    tk.group([TR(psb[b][:, HS[h]], kT.ap[:, h, CS[c]], identb[:]) for h in range(H)], r=kT.k() + ["identb"], w=PK(b))
                    tk.op("dve", I("tensor_tensor", out=kz.ap[:, u, :], in0=psb[b][:, 0:512], in1=con[:, C_ZTAB:C_ZTAB + 512], op=ALU.mult),
                          r=PK(b) + ["con"], w=kz.k(u))
                    b = nb()
                    tk.group([MM(ps[b][:, HS[h]], kT.ap[:, h, CS[c]], qT.ap[:, h, CS[c]], True, True) for h in range(H)], r=kT.k() + qT.k(), w=PK(b))
                    tk.op("dve", I("tensor_tensor", out=smk.ap[:, u, :], in0=ps[b][:, :], in1=con[:, C_DMASK:C_DMASK + 512], op=ALU.mult),
                          r=PK(b) + ["con"], w=smk.k(u))

                def ret_KIX(c, l=l):
                    u = c % 2
                    rb_ap, rb_k = retbuf(c)
                    bk = nb()
                    tk.group([MM(ps[bk][:, HS[h]], kz.ap[:, u, HS[h]], vtok.ap[:, c, HS[h]], True, True) for h in range(H)],
                             r=kz.k(u) + vtok.k(c), w=PK(bk))
                    bi = nb()
                    tk.group([MM(ps[bi][:, HS[h]], smk.ap[:, u, HS[h]], vtok.ap[:, c, HS[h]], True, True) for h in range(H)],
                             r=smk.k(u) + vtok.k(c), w=PK(bi))
                    bc = nb()
                    tk.group([MM(ps[bc][:, HS[h]], qT.ap[:, h, CS[c]], Sbf[:, l, HS[h]], True, True) for h in range(H)],
                             r=qT.k() + [f"Sbf{l}"], w=PK(bc))
                    for h in range(H):
                        tk.op("dve", I("scalar_tensor_tensor", out=Sst[:, l, HS[h]], in0=Sst[:, l, HS[h]], scalar=CD[h], in1=ps[bk][:, HS[h]],
                                       op0=ALU.mult, op1=ALU.add), r=PK(bk) + [f"S{l}"], w=[f"S{l}"])
                    tk.op("act", I("activation", out=Sbf[:, l, :], in_=Sst[:, l, :], func=AF.Copy), r=[f"S{l}"], w=[f"Sbf{l}"])
                    tk.op("act", I("activation", out=innsb.ap[:, u, :], in_=ps[bi][:, :], func=AF.Copy), r=PK(bi), w=innsb.k(u))
                    for h in range(H):
                        tk.op("dve", I("scalar_tensor_tensor", out=rb_ap[:, HS[h]], in0=ps[bc][:, HS[h]], scalar=con[:, C_XI + h:C_XI + h + 1],
                                       in1=innsb.ap[:, u, HS[h]], op0=ALU.mult, op1=ALU.add),
                              r=PK(bc) + innsb.k(u) + ["con"], w=rb_k)

                def ret_GN(c):
                    rb_ap, rb_k = retbuf(c)
                    for h in range(H):
                        tk.op("dve", I("bn_stats", out=stats[:, c, h, :], in_=rb_ap[:, HS[h]]), r=rb_k, w=[("stats", c, h)])
                        tk.op("dve", I("bn_aggr", out=mv[:, c, h, :], in_=stats[:, c, h, :]), r=[("stats", c, h)], w=[("mv", c)])
                    tk.op("dve", I("tensor_scalar", out=veps[:, c, :], in0=mv[:, c, :, 1], scalar1=LN_EPS, scalar2=None, op0=ALU.add),
                          r=[("mv", c)], w=[("veps", c)])
                    tk.op("pool", I("tensor_tensor", out=rstd4[:, c, :], in0=veps[:, c, :], in1=mhalf[:], op=ALU.pow),
                          r=[("veps", c), "mhalf"], w=[("rstd4", c)])
                    tk.op("pool", I("tensor_tensor", out=nmr[:, c, :], in0=mv[:, c, :, 0], in1=rstd4[:, c, :], op=ALU.mult),
                          r=[("mv", c), ("rstd4", c)], w=[("nmr", c)])
                    tk.op("pool", I("tensor_scalar", out=nmr[:, c, :], in0=nmr[:, c, :], scalar1=-1.0, scalar2=0.0, op0=ALU.mult, op1=ALU.add),
                          r=[("nmr", c)], w=[("nmr", c)])
                    for h in range(H):
                        tk.op("pool", I("tensor_scalar", out=rn4[:, c, HS[h]], in0=rb_ap[:, HS[h]], scalar1=rstd4[:, c, h:h + 1],
                                        scalar2=nmr[:, c, h:h + 1], op0=ALU.mult, op1=ALU.add),
                              r=rb_k + [("rstd4", c), ("nmr", c)], w=rnk(c))

                def ret_OUT(c, l=l):
                    u = c % 2
                    bt = nb()
                    tk.group([TR(psb[bt][:, HS[h]], rn4[:, c, HS[h]], identb[:]) for h in range(H)], r=rnk(c) + ["identb"], w=PK(bt))
                    for h in range(H):
                        tk.op("act", I("activation", out=gtmp.ap[:, u, HS[h]], in_=psb[bt][:, HS[h]], func=AF.Identity,
                                       scale=pc(l, "gng", h), bias=pc(l, "gnb", h)), r=PK(bt) + ["par"], w=gtmp.k(u))
                    tk.op("dve", I("tensor_tensor", out=mixT[:, 4:8, CS[c]], in0=gtmp.ap[:, u, :].rearrange("p (h q) -> p h q", h=4),
                                    in1=sgT.ap[:, :, CS[c]], op=ALU.mult),
                          r=gtmp.k(u) + sgT.k(), w=[("mixT", 4), ("mixT", 5), ("mixT", 6), ("mixT", 7)])

                ret_TS(0)
                ret_TS(1)
                ret_KIX(0)
                conv_tail_B()
                ret_TS(2)
                ret_KIX(1)
                conv_tail_C()
                ret_TS(3)
                ret_GN(0)
                ret_KIX(2)
                ret_GN(1)
                ret_KIX(3)
                conv_tail_D()
                ret_GN(2)
                ret_GN(3)
                tk.label = 'win_g'
                sl_g = ring_load(l, 7)

                def ev_g(mi_, b):
                    tk.op("act", I("activation", out=sgT.ap[:, mi_, :], in_=ps[b][:, :], func=AF.Silu), r=PK(b), w=sgT.k(mi_))
                linear_fm(sl_g, rhs_h, hT_keys, KC, ev_g)
                tk.label = 'ret'
                ret_OUT(0)
                ret_OUT(1)
                ret_OUT(2)
                ret_OUT(3)
                nxt = (ti, l + 1) if l + 1 < depth else (ti + 1, 0)
                if nxt[0] < NT and depth > 1:
                    emit_diag(nxt[1])
                tk.op("act", I("activation", out=tabw[:, 0:1], in_=tabw[:, 1:2], func=AF.Ln), r=[], w=["tabw"])

                if STAGE < 7:
                    continue
                tk.label = 'wout'
                mix_keys = [("mixT", kc) for kc in range(KC)]
                for half in range(2):
                    sl = ring_load(l, 8 + half)
                    linear_fm(sl, lambda kc: mixT[:, kc, :], mix_keys, KC, lambda mi_, b, half=half: residual_evac(half * 4 + mi_, b))

                if STAGE < 8:
                    continue
                if first:
                    emit_casts(gi + 10)
                tk.label = 'norm2'
                emit_norm(l, "gffn")
                if l == depth - 1 and ti + 1 < NT:
                    emit_tile_prefetch(ti + 1)
                tk.label = 'ff1'
                for pi in range(8):
                    sl = ring_load(l, 10 + pi)

                    def ev_ff1(mi_, b, pi=pi):
                        j = pi * 4 + mi_
                        u = j % 2
                        tk.op("act", I("activation", out=sqf.ap[:, u, :], in_=ps[b][:, :], func=AF.Square), r=PK(b), w=sqf.k(u))
                        tk.op("dve", I("scalar_tensor_tensor", out=hid.ap[:, j, :], in0=ps[b][:, :], scalar=0.0, in1=sqf.ap[:, u, :],
                                       op0=ALU.is_gt, op1=ALU.mult), r=PK(b) + sqf.k(u), w=hid.k(j))
                    if pi == 0:
                        linear_first([(sl, mi_) for mi_ in range(4)], rhs_h, hT_keys, KC, [ev_ff1] * 4)
                    else:
                        linear_fm(sl, rhs_h, hT_keys, KC, ev_ff1)
                tk.label = 'ff2'
                for half in range(2):
                    banks = [nb() for _ in range(4)]
                    for jb in range(4):
                        sl = ring_load(l, 18 + half * 4 + jb)
                        if jb < 3:
                            fns = []
                            for jj in range(8):
                                for mi in range(4):
                                    fns.append((MM(ps[banks[mi]][:, :], ring[sl][:, jj * 512 + mi * 128: jj * 512 + mi * 128 + 128],
                                                   hid.ap[:, jb * 8 + jj, :], jb == 0 and jj == 0, False), hid.k(jb * 8 + jj)))
                            tk.group(fns, r=[("ring", sl)], w=[("ps", bk_) for bk_ in banks])
                        else:
                            for mi in range(4):
                                tk.group([MM(ps[banks[mi]][:, :], ring[sl][:, jj * 512 + mi * 128: jj * 512 + mi * 128 + 128],
                                             hid.ap[:, jb * 8 + jj, :], False, jj == 7) for jj in range(8)],
                                         r=[("ring", sl)] + hid.k(jb * 8, 8), w=PK(banks[mi]))
                                residual_evac(half * 4 + mi, banks[mi])

                if STAGE < 9:
                    continue
                if first and l + 1 < depth:
                    emit_casts(gi + NG + 4)
                tk.label = 'ple'
                tk.dma("sp", I("dma_start", out=p_in.ap, in_=p_d[l, t0:t0 + T, :].rearrange("(c p) f -> p c f", p=128)), s_p, w=p_in.k())
                for k2 in range(2):
                    b = nb()
                    tk.group([TR(ps[b][:, c * 128:(c + 1) * 128], p_in.ap[:, c, k2 * 128:(k2 + 1) * 128], identf) for c in range(NCH)],
                             r=p_in.k() + ["con"], w=PK(b))
                    tk.op("act", I("activation", out=pT.ap[:, k2, :], in_=ps[b][:, :], func=AF.Copy, scale=0.5), r=PK(b), w=pT.k(k2))
                tk.label = 'norm3'
                emit_norm(l, "gple")
                tk.label = 'ple'
                sl_pp = ring_load(l, 28)

                def ple_tail(m, bg, sl_pp=sl_pp):
                    u = m % 2
                    tk.op("act", I("activation", out=thg.ap[:, u, :], in_=ps[bg][:, :], func=AF.Tanh, scale=0.5), r=PK(bg), w=thg.k(u))
                    b = nb()
                    tk.group([MM(ps[b][:, :], ring[sl_pp][:, k2 * 1024 + m * 128: k2 * 1024 + m * 128 + 128], pT.ap[:, k2, :], k2 == 0, k2 == 1)
                              for k2 in range(2)], r=[("ring", sl_pp)] + pT.k(), w=PK(b))
                    tk.op("dve", I("scalar_tensor_tensor", out=ps[b][:, :], in0=thg.ap[:, u, :], scalar=1.0, in1=ps[b][:, :],
                                   op0=ALU.add, op1=ALU.mult), r=thg.k(u) + PK(b), w=PK(b))
                    tk.op("dve", I("tensor_tensor", out=xT[:, m, :], in0=xT[:, m, :], in1=ps[b][:, :], op=ALU.add),
                          r=[("xT", m)] + PK(b), w=[("xT", m)])
                    tk.op("act", I("activation", out=sq[:, m, :], in_=xT[:, m, :], func=AF.Square), r=[("xT", m)], w=[("sq", m)])

                for half in range(2):
                    sl = ring_load(l, 26 + half)
                    if half == 0:
                        linear_first([(sl, mi_) for mi_ in range(4)], rhs_h, hT_keys, KC, [lambda mi_, bg: ple_tail(mi_, bg)] * 4)
                    else:
                        for mi in range(4):
                            linear_fm(sl, rhs_h, hT_keys, KC, lambda mi_, bg: ple_tail(4 + mi_, bg), mi_list=[mi])
                tk.op("act", I("activation", out=tabw[:, 0:1], in_=tabw[:, 1:2], func=AF.Ln), r=[], w=["tabw"])
                if first and l + 1 < depth:
                    emit_casts(gi + NG + 5)

            tk.label = 'final'
            if 'min0' in FLAGS or 'min1' in FLAGS:
                tk.dma("sp", I("dma_start", out=out_d[t0:t0 + T, :].rearrange("(c p) f -> p c f", p=128), in_=xstage.ap), s_o, r=xstage.k() + [("xT", m) for m in range(KC)] + [("sq", m) for m in range(KC)], w=["outd"])
                continue
            b = emit_rstd()
            if 'min2' in FLAGS:
                tk.dma("sp", I("dma_start", out=out_d[t0:t0 + T, :].rearrange("(c p) f -> p c f", p=128), in_=xstage.ap), s_o, r=xstage.k() + PK(b), w=["outd"])
                continue
            for m in range(KC):
                u = m % 2
                tk.op("dve", I("scalar_tensor_tensor", out=obuf.ap[:, u, :], in0=xT[:, m, :], scalar=par[:, 2 * PL + m:2 * PL + m + 1],
                               in1=ps[b][:, :], op0=ALU.mult, op1=ALU.mult), r=[("xT", m), "par"] + PK(b), w=obuf.k(u))
                bt = nb()
                tk.group([TR(ps[bt][:, c * 128:(c + 1) * 128], obuf.ap[:, u, c * 128:(c + 1) * 128], identf) for c in range(NCH)],
                         r=obuf.k(u) + ["con"], w=PK(bt))
                tk.op("act", I("activation", out=ostage.ap[:, :, m * 128:(m + 1) * 128], in_=ps[bt][:, :].rearrange("p (c f) -> p c f", c=4), func=AF.Copy),
                      r=PK(bt), w=ostage.k())
            oq = "sp" if "spout" in FLAGS else "act"
            tk.dma(oq, I("dma_start", out=out_d[t0:t0 + T, :].rearrange("(c p) f -> p c f", p=128), in_=ostage.ap), s_o, r=ostage.k(), w=["outd"])
        tk.final_wait("sp" if "spout" in FLAGS else "act", s_o)
        block = es.enter_context(nc.Block())
        tk.replay(block)
        if DUMP_LABELS:
            import json
            json.dump(tk.pe_labels, open(DUMP_LABELS, 'w'))
        print(f"[build] ops={tk.nops} waits={tk.nwait} sems={len(tk.sem)} banks_used={bank_ctr[0]}", flush=True)
    return nc


def host_consts():
    c = np.zeros((128, NCONST), np.float32)
    half = 64
    inv = (1.0 / (np.float32(10000.0) ** (np.arange(0, half, dtype=np.float32) * np.float32(2.0 / 128)))).astype(np.float32)
    c[:, C_INVF] = np.concatenate([inv, inv])
    c[:64, C_SIGN] = -1.0
    c[64:, C_SIGN] = 1.0
    idx = np.arange(128, dtype=np.float64)
    for h in range(H):
        lg = np.log(1.0 - 2.0 ** (-5.0 - h))
        c[:, C_XI + h] = np.exp((idx + 1.0) * lg)
        diff = idx[None, :] - idx[:, None]
        dm = np.where(diff >= 0, np.exp(np.maximum(diff, 0.0) * lg), 0.0) * (128.0 ** -0.5)
        c[:, C_DMASK + h * 128:C_DMASK + (h + 1) * 128] = dm
        zeta = np.exp((127.0 - idx) * lg) * (128.0 ** -0.5)
        c[:, C_ZTAB + h * 128:C_ZTAB + (h + 1) * 128] = zeta[:, None]
    c[:, C_IDENT:C_IDENT + 128] = np.eye(128, dtype=np.float32)
    for m in range(128):
        c[(m + 64) % 128, C_PSWAP + m] = 1.0
    return c


def host_params(inp):
    P = np.zeros((128, NPAR), np.float32)

    def cols(v, n):
        return np.asarray(v, np.float32).reshape(n, 128).T

    for l in range(DEPTH):
        b = l * PL
        P[:, b + 0:b + 8] = cols(inp["norm_mix_g"][l], 8)
        P[:, b + 8:b + 16] = cols(inp["norm_ffn_g"][l], 8)
        P[:, b + 16:b + 24] = cols(inp["norm_ple_g"][l], 8)
        P[:, b + 24:b + 28] = cols(inp["conv_dw_b"][l], 4)
        P[:, b + 28:b + 32] = cols(inp["conv_ln_g"][l], 4)
        P[:, b + 32:b + 36] = cols(inp["conv_ln_b"][l], 4)
        P[:, b + 36:b + 40] = cols(inp["ret_gn_g"][l], 4)
        P[:, b + 40:b + 44] = cols(inp["ret_gn_b"][l], 4)
        w = np.asarray(inp["conv_dw_w"][l], np.float32)
        P[:, b + 44:b + 168] = w.reshape(CW, 4, 128).transpose(2, 0, 1).reshape(128, CW * 4)
    P[:, 2 * PL:2 * PL + 8] = cols(inp["final_norm_g"], 8)
    return P


_NC_CACHE = {}


def run(inp, S, n_cores):
    if S not in _NC_CACHE:
        _NC_CACHE[S] = build(S)
    nc = _NC_CACHE[S]
    consts = host_consts()
    params = host_params(inp)
    f = lambda k: np.ascontiguousarray(np.asarray(inp[k], np.float32))
    shared = {"params": params, "consts": consts, "w_in": f("w_in"), "w_out": f("w_out"), "w_ff1": f("w_ff1"),
              "w_ff2": f("w_ff2"), "w_pg": f("w_ple_gate"), "w_pp": f("w_ple_proj")}
    x = np.asarray(inp["x"], np.float32)
    p = np.asarray(inp["p"], np.float32)
    pos = np.asarray(inp["positions"], np.int32)
    in_maps = []
    for b in range(n_cores):
        m = dict(shared)
        m["x"] = np.ascontiguousarray(x[b, :S])
        m["p"] = np.ascontiguousarray(p[:, b, :S])
        m["pos"] = np.ascontiguousarray(pos[b:b + 1, :S])
        in_maps.append(m)
    res = run_bass_kernel_spmd(nc, in_maps, core_ids=list(range(n_cores)))
    return np.stack([r["out"] for r in res.results], axis=0)


def kernel(**inputs):
    return run(inputs, SEQ, BATCH).astype(np.float32)
```
